# Optimizing a Trainium2 kernel written in Bass

```python
import math
import functools
import jax
import jax.numpy as jnp
from jax import lax
import numpy as np

D_MODEL = 2048
BATCH = 32
SEQ = 256
DEPTH = 2
DEC_BATCH = 2
DEC_SEQ = 4096
PAST_LEN = 256

GRID_W = 64
N_BRANCH = 4
BR_W = D_MODEL // 4
CONV_W = 4
RG_W = BR_W
RG_HEADS = 4
RG_HD = RG_W // RG_HEADS
RG_C = 8.0
POOL_WINDOWS = (2, 4, 8, 16)
POOL_G = BR_W // len(POOL_WINDOWS)
SGU_W = BR_W
SGU_HEADS = 4
SGU_HD = SGU_W // SGU_HEADS
SGU_CHUNK = 128
DN_DK = 128
DN_DV = 128
DN_HEADS = BR_W // DN_DV
DN_W = DN_HEADS * DN_DV
DN_QKV = DN_HEADS * (2 * DN_DK + DN_DV)
DN_CHUNK = 64
OFF_RG_X = 0
OFF_RG_G = OFF_RG_X + RG_W
OFF_POOL = OFF_RG_G + RG_W
OFF_SGU = OFF_POOL + BR_W
OFF_DN_QKV = OFF_SGU + 2 * SGU_W
OFF_DN_Z = OFF_DN_QKV + DN_QKV
OFF_DN_BA = OFF_DN_Z + DN_W
OFF_GATE = OFF_DN_BA + 2 * 2 * DN_HEADS
N_IN = OFF_GATE + N_BRANCH * D_MODEL
D_FF = 11 * D_MODEL // 4
N_EXPERTS = 8
TOP_K = 2
D_FF_EXPERT = D_FF // 2
N_DENSE = (DEPTH + 1) // 2
N_MOE = DEPTH // 2
EPS = 1e-6
F32 = jnp.float32

kernel_name = 'hybrid_flow_rglru_pool_gmlp_deltanet'


def rms_norm(x, g):
    xf = x.astype(F32)
    y = xf * lax.rsqrt(jnp.mean(xf * xf, axis=-1, keepdims=True) + EPS)
    return (y * g.astype(F32)).astype(x.dtype)


def causal_dwconv(x, w):
    L = x.shape[1]
    xp = jnp.pad(x, ((0, 0), (CONV_W - 1, 0), (0, 0)))
    return sum(xp[:, j:j + L] * w[j] for j in range(CONV_W))


def linear_scan(a, b, h0):
    b = b.at[:, 0].add(a[:, 0] * h0)

    def combine(left, right):
        return left[0] * right[0], right[0] * left[1] + right[1]

    _, h = lax.associative_scan(combine, (a, b), axis=1)
    return h


def rglru_direction(x, conv_w, conv_b, wa, ba, wx, bx, lam, h0):
    bsz, L, _ = x.shape
    xc = causal_dwconv(x, conv_w) + conv_b
    xh = xc.reshape(bsz, L, RG_HEADS, RG_HD)
    r = jax.nn.sigmoid(jnp.einsum('blhi,hij->blhj', xh, wa).reshape(bsz, L, RG_W).astype(F32) + ba.astype(F32))
    i = jax.nn.sigmoid(jnp.einsum('blhi,hij->blhj', xh, wx).reshape(bsz, L, RG_W).astype(F32) + bx.astype(F32))
    log_a = -RG_C * jax.nn.softplus(-lam.astype(F32)) * r
    a = jnp.exp(log_a)
    b = jnp.sqrt(-jnp.expm1(2.0 * log_a)) * i * xc.astype(F32)
    h = linear_scan(a, b, h0.astype(F32))
    return h, h[:, -1]


def rglru_mixer(xr, xg, conv_w, conv_b, wa, ba, wx, bx, lam, h0):
    hf, sf = rglru_direction(xr, conv_w[0], conv_b[0], wa[0], ba[0], wx[0], bx[0], lam[0], h0[:, 0])
    hb, sb = rglru_direction(xr[:, ::-1], conv_w[1], conv_b[1], wa[1], ba[1], wx[1], bx[1], lam[1], h0[:, 1])
    y = jax.nn.gelu(xg.astype(F32)) * (hf + hb[:, ::-1])
    return y.astype(xr.dtype), jnp.stack([sf, sb], axis=1)


def pool_mixer(x, w_pool, scale):
    n = x.shape[-2]
    xf = x.astype(F32)
    cs = jnp.cumsum(xf, axis=-2)
    cs = jnp.concatenate([jnp.zeros_like(cs[..., :1, :]), cs], axis=-2)
    t = jnp.arange(n)
    outs = []
    for j, w in enumerate(POOL_WINDOWS):
        lo = jnp.maximum(t - w // 2, 0)
        hi = jnp.minimum(t + (w - 1 - w // 2), n - 1)
        csj = cs[..., j * POOL_G:(j + 1) * POOL_G]
        s = jnp.take(csj, hi + 1, axis=-2) - jnp.take(csj, lo, axis=-2)
        cnt = (hi - lo + 1).astype(F32)[:, None]
        outs.append(s / cnt - xf[..., j * POOL_G:(j + 1) * POOL_G])
    pooled = jnp.stack(outs, axis=-2)
    y = jnp.einsum('...ngc,gcd->...ngd', pooled, w_pool.astype(F32))
    return (y.reshape(x.shape) * scale.astype(F32)).astype(x.dtype)


def sgu_mixer(z, ln_g, ln_b, ws, bs):
    bsz, L, _ = z.shape
    u = z[..., :SGU_W].astype(F32)
    v = z[..., SGU_W:].astype(F32)
    mu = jnp.mean(v, axis=-1, keepdims=True)
    var = jnp.mean(jnp.square(v - mu), axis=-1, keepdims=True)
    v = (v - mu) * lax.rsqrt(var + EPS) * ln_g.astype(F32) + ln_b.astype(F32)
    vc = v.reshape(bsz, L // SGU_CHUNK, SGU_CHUNK, SGU_HEADS, SGU_HD)
    s = jnp.einsum('bnphc,hqp->bnqhc', vc, ws.astype(F32)) + bs.astype(F32).T[:, :, None]
    return (u * s.reshape(bsz, L, SGU_W)).astype(z.dtype)


def l2norm(x):
    return x * lax.rsqrt(jnp.sum(x * x, axis=-1, keepdims=True) + EPS)


def chunk_gated_delta(q, k, v, g, beta, s0):
    bsz, L, H, _ = q.shape
    V = v.shape[-1]
    C = DN_CHUNK
    N = L // C

    def chunks(t):
        return jnp.moveaxis(t.reshape(bsz, N, C, H, *t.shape[3:]), 3, 1)

    q, k, v, beta = chunks(q), chunks(k), chunks(v), chunks(beta)
    g = jnp.cumsum(chunks(g), axis=-1)
    incl = jnp.tril(jnp.ones((C, C), bool))
    strict = jnp.tril(jnp.ones((C, C), bool), -1)
    decay = jnp.exp(jnp.where(incl, g[..., :, None] - g[..., None, :], -jnp.inf))
    kb = k * beta[..., None]
    a_mat = jnp.where(strict, jnp.einsum('bhnck,bhndk->bhncd', kb, k) * decay, 0.0) + jnp.eye(C, dtype=F32)
    rhs = jnp.concatenate([v * beta[..., None], kb * jnp.exp(g)[..., None]], axis=-1)
    w = lax.linalg.triangular_solve(a_mat, rhs, left_side=True, lower=True, unit_diagonal=True)
    val, kcum = w[..., :V], w[..., V:]
    qk = jnp.einsum('bhnck,bhndk->bhncd', q, k) * decay
    qg = q * jnp.exp(g)[..., None]
    kend = k * jnp.exp(g[..., -1:] - g)[..., None]
    gend = jnp.exp(g[..., -1])

    def step(s, xs):
        qk_i, qg_i, kc_i, val_i, ke_i, ge_i = xs
        u = val_i - jnp.einsum('bhck,bhkv->bhcv', kc_i, s)
        o = jnp.einsum('bhck,bhkv->bhcv', qg_i, s) + jnp.einsum('bhcd,bhdv->bhcv', qk_i, u)
        s = s * ge_i[..., None, None] + jnp.einsum('bhck,bhcv->bhkv', ke_i, u)
        return s, o

    xs = tuple(jnp.moveaxis(t, 2, 0) for t in (qk, qg, kcum, val, kend, gend))
    s_fin, o = lax.scan(step, s0, xs)
    o = jnp.transpose(o, (1, 0, 3, 2, 4)).reshape(bsz, L, H, V)
    return o, s_fin


def dn_direction(qkv, b_logit, a_logit, conv_w, a_log, dt_bias, s0):
    bsz, L, _ = qkv.shape
    hk = DN_HEADS * DN_DK
    y = jax.nn.silu(causal_dwconv(qkv, conv_w)).astype(F32)
    q = l2norm(y[..., :hk].reshape(bsz, L, DN_HEADS, DN_DK)) * (DN_DK ** -0.5)
    k = l2norm(y[..., hk:2 * hk].reshape(bsz, L, DN_HEADS, DN_DK))
    v = y[..., 2 * hk:].reshape(bsz, L, DN_HEADS, DN_DV)
    beta = jax.nn.sigmoid(b_logit.astype(F32))
    g = -jnp.exp(a_log.astype(F32)) * jax.nn.softplus(a_logit.astype(F32) + dt_bias.astype(F32))
    return chunk_gated_delta(q, k, v, g, beta, s0.astype(F32))


def dn_mixer(qkv, z, ba, conv_w, a_log, dt_bias, norm_g, s0):
    bsz, L, _ = qkv.shape
    ba = ba.reshape(bsz, L, 2, 2, DN_HEADS)
    o_f, s_f = dn_direction(qkv, ba[:, :, 0, 0], ba[:, :, 0, 1], conv_w[0], a_log[0], dt_bias[0], s0[:, 0])
    rev = ba[:, ::-1]
    o_b, s_b = dn_direction(qkv[:, ::-1], rev[:, :, 1, 0], rev[:, :, 1, 1], conv_w[1], a_log[1], dt_bias[1], s0[:, 1])
    o = o_f + o_b[:, ::-1]
    o = o * lax.rsqrt(jnp.mean(o * o, axis=-1, keepdims=True) + EPS) * norm_g.astype(F32)
    o = o.reshape(bsz, L, DN_W) * jax.nn.silu(z.astype(F32))
    return o.astype(qkv.dtype), jnp.stack([s_f, s_b], axis=1)


def mixer_block(h, lp, rg_h0, dn_s0, grid):
    bsz, L, _ = h.shape
    proj = jnp.einsum('bld,dn->bln', h, lp['w_in'])
    y_a, rg_state = rglru_mixer(proj[..., OFF_RG_X:OFF_RG_X + RG_W], proj[..., OFF_RG_G:OFF_RG_G + RG_W],
                                lp['rg_conv_w'], lp['rg_conv_b'], lp['rg_wa'], lp['rg_ba'],
                                lp['rg_wx'], lp['rg_bx'], lp['rg_lam'], rg_h0)
    pin = proj[..., OFF_POOL:OFF_POOL + BR_W]
    if grid:
        rows = L // GRID_W
        y_b = pool_mixer(pin.reshape(bsz, rows, GRID_W, BR_W), lp['pool_w'], lp['pool_scale']).reshape(bsz, L, BR_W)
    else:
        y_b = pool_mixer(pin, lp['pool_w'], lp['pool_scale'])
    y_c = sgu_mixer(jax.nn.gelu(proj[..., OFF_SGU:OFF_SGU + 2 * SGU_W]),
                    lp['sgu_ln_g'], lp['sgu_ln_b'], lp['sgu_ws'], lp['sgu_bs'])
    y_d, dn_state = dn_mixer(proj[..., OFF_DN_QKV:OFF_DN_QKV + DN_QKV], proj[..., OFF_DN_Z:OFF_DN_Z + DN_W],
                             proj[..., OFF_DN_BA:OFF_GATE], lp['dn_conv_w'], lp['dn_a_log'],
                             lp['dn_dt_bias'], lp['dn_norm_g'], dn_s0)
    ys = jnp.stack([y_a, y_b, y_c, y_d], axis=2)
    br = jnp.einsum('blkc,kcd->blkd', ys, lp['w_br'])
    gates = jax.nn.sigmoid(proj[..., OFF_GATE:].reshape(bsz, L, N_BRANCH, D_MODEL))
    merged = jnp.sum(gates * br, axis=2)
    return merged @ lp['w_out'], rg_state, dn_state


def swiglu(x, w1, w3, w2):
    return (jax.nn.silu(x @ w1) * (x @ w3)) @ w2


def moe_swiglu(x, wr, br, w1, w3, w2):
    shp = x.shape
    xt = x.reshape(-1, shp[-1])
    logits = (xt @ wr).astype(F32) + br.astype(F32)
    top_v, top_i = lax.top_k(logits, TOP_K)
    wts = jax.nn.softmax(top_v, axis=-1)
    gate = jnp.sum(jax.nn.one_hot(top_i, N_EXPERTS, dtype=F32) * wts[..., None], axis=1)
    out = jnp.zeros(xt.shape, F32)
    for e in range(N_EXPERTS):
        out = out + gate[:, e:e + 1] * swiglu(xt, w1[e], w3[e], w2[e]).astype(F32)
    return out.astype(x.dtype).reshape(shp)


def trunk_layer(x, mod, lp, ffn, rg_h0, dn_s0, grid):
    sh1, sc1, g1, sh2, sc2, g2 = jnp.split(mod, 6, axis=-1)
    h = rms_norm(x, lp['norm1_g']) * (1.0 + sc1) + sh1
    mix, rg_s, dn_s = mixer_block(h, lp, rg_h0, dn_s0, grid)
    x = x + g1 * mix
    h = rms_norm(x, lp['norm2_g']) * (1.0 + sc2) + sh2
    x = x + g2 * ffn(h)
    return x, rg_s, dn_s


def setup_inputs(seed: int = 0) -> dict:
    key = jax.random.key(seed)
    ks = iter(jax.random.split(key, 64))

    def nrm(shape, scale=1.0):
        return scale * jax.random.normal(next(ks), shape, F32)

    def gain(shape):
        return 1.0 + 0.05 * nrm(shape)

    u = jax.random.uniform(next(ks), (DEPTH, 2, RG_W), F32, 0.9, 0.999)
    s = u ** (1.0 / RG_C)
    rg_lam = jnp.log(s) - jnp.log1p(-s)
    dn_a_log = jnp.log(jax.random.uniform(next(ks), (DEPTH, 2, DN_HEADS), F32, 1.0, 16.0))
    dt = jnp.exp(jax.random.uniform(next(ks), (DEPTH, 2, DN_HEADS), F32, math.log(1e-3), math.log(0.1)))
    dn_dt_bias = dt + jnp.log(-jnp.expm1(-dt))
    return {
        'x_prompt': nrm((BATCH, SEQ, D_MODEL)),
        'x_sample': nrm((DEC_BATCH, DEC_SEQ, D_MODEL)),
        'state_rglru': nrm((DEC_BATCH, DEPTH, 2, RG_W)),
        'state_delta': nrm((DEC_BATCH, DEPTH, 2, DN_HEADS, DN_DK, DN_DV), DN_DK ** -0.5),
        'c': nrm((DEC_BATCH, D_MODEL)),
        'c_ctx': nrm((D_MODEL,)),
        'ada_w': nrm((DEPTH, D_MODEL, 6 * D_MODEL), 0.5 * D_MODEL ** -0.5),
        'ada_b': nrm((DEPTH, 6 * D_MODEL), 0.01),
        'norm1_g': gain((DEPTH, D_MODEL)),
        'norm2_g': gain((DEPTH, D_MODEL)),
        'w_in': nrm((DEPTH, D_MODEL, N_IN), D_MODEL ** -0.5),
        'rg_conv_w': nrm((DEPTH, 2, CONV_W, RG_W), CONV_W ** -0.5),
        'rg_conv_b': nrm((DEPTH, 2, RG_W), 0.01),
        'rg_wa': nrm((DEPTH, 2, RG_HEADS, RG_HD, RG_HD), RG_HD ** -0.5),
        'rg_ba': nrm((DEPTH, 2, RG_W), 0.01),
        'rg_wx': nrm((DEPTH, 2, RG_HEADS, RG_HD, RG_HD), RG_HD ** -0.5),
        'rg_bx': nrm((DEPTH, 2, RG_W), 0.01),
        'rg_lam': rg_lam,
        'pool_w': nrm((DEPTH, len(POOL_WINDOWS), POOL_G, POOL_G), POOL_G ** -0.5),
        'pool_scale': gain((DEPTH, BR_W)),
        'sgu_ln_g': gain((DEPTH, SGU_W)),
        'sgu_ln_b': nrm((DEPTH, SGU_W), 0.01),
        'sgu_ws': nrm((DEPTH, SGU_HEADS, SGU_CHUNK, SGU_CHUNK), SGU_CHUNK ** -0.5),
        'sgu_bs': 1.0 + nrm((DEPTH, SGU_HEADS, SGU_CHUNK), 0.01),
        'dn_conv_w': nrm((DEPTH, 2, CONV_W, DN_QKV), CONV_W ** -0.5),
        'dn_a_log': dn_a_log,
        'dn_dt_bias': dn_dt_bias,
        'dn_norm_g': gain((DEPTH, DN_DV)),
        'w_br': nrm((DEPTH, N_BRANCH, BR_W, D_MODEL), BR_W ** -0.5),
        'w_out': nrm((DEPTH, D_MODEL, D_MODEL), D_MODEL ** -0.5),
        'ffn_w1': nrm((N_DENSE, D_MODEL, D_FF), D_MODEL ** -0.5),
        'ffn_w3': nrm((N_DENSE, D_MODEL, D_FF), D_MODEL ** -0.5),
        'ffn_w2': nrm((N_DENSE, D_FF, D_MODEL), D_FF ** -0.5),
        'moe_wr': nrm((N_MOE, D_MODEL, N_EXPERTS), D_MODEL ** -0.5),
        'moe_br': nrm((N_MOE, N_EXPERTS), 0.01),
        'moe_w1': nrm((N_MOE, N_EXPERTS, D_MODEL, D_FF_EXPERT), D_MODEL ** -0.5),
        'moe_w3': nrm((N_MOE, N_EXPERTS, D_MODEL, D_FF_EXPERT), D_MODEL ** -0.5),
        'moe_w2': nrm((N_MOE, N_EXPERTS, D_FF_EXPERT, D_MODEL), D_FF_EXPERT ** -0.5),
        'final_g': gain((D_MODEL,)),
    }


def reference(x_prompt, x_sample, state_rglru, state_delta, c, c_ctx, ada_w, ada_b, norm1_g, norm2_g,
              w_in, rg_conv_w, rg_conv_b, rg_wa, rg_ba, rg_wx, rg_bx, rg_lam, pool_w, pool_scale,
              sgu_ln_g, sgu_ln_b, sgu_ws, sgu_bs, dn_conv_w, dn_a_log, dn_dt_bias, dn_norm_g,
              w_br, w_out, ffn_w1, ffn_w3, ffn_w2, moe_wr, moe_br, moe_w1, moe_w3, moe_w2, final_g):
    bp = x_prompt.shape[0]
    rg_zero = jnp.zeros((bp, 2, RG_W), F32)
    dn_zero = jnp.zeros((bp, 2, DN_HEADS, DN_DK, DN_DV), F32)
    xp, xs = x_prompt, x_sample
    rg_new, dn_new = [], []
    for l in range(DEPTH):
        lp = {
            'norm1_g': norm1_g[l], 'norm2_g': norm2_g[l], 'w_in': w_in[l],
            'rg_conv_w': rg_conv_w[l], 'rg_conv_b': rg_conv_b[l], 'rg_wa': rg_wa[l], 'rg_ba': rg_ba[l],
            'rg_wx': rg_wx[l], 'rg_bx': rg_bx[l], 'rg_lam': rg_lam[l],
            'pool_w': pool_w[l], 'pool_scale': pool_scale[l],
            'sgu_ln_g': sgu_ln_g[l], 'sgu_ln_b': sgu_ln_b[l], 'sgu_ws': sgu_ws[l], 'sgu_bs': sgu_bs[l],
            'dn_conv_w': dn_conv_w[l], 'dn_a_log': dn_a_log[l], 'dn_dt_bias': dn_dt_bias[l],
            'dn_norm_g': dn_norm_g[l], 'w_br': w_br[l], 'w_out': w_out[l],
        }
        j = l // 2
        if l % 2 == 0:
            ffn = functools.partial(swiglu, w1=ffn_w1[j], w3=ffn_w3[j], w2=ffn_w2[j])
        else:
            ffn = functools.partial(moe_swiglu, wr=moe_wr[j], br=moe_br[j], w1=moe_w1[j], w3=moe_w3[j], w2=moe_w2[j])
        mod_ctx = (jax.nn.silu(c_ctx) @ ada_w[l] + ada_b[l])[None, None, :]
        mod_lat = (jax.nn.silu(c) @ ada_w[l] + ada_b[l])[:, None, :]
        xp, rg_s, dn_s = trunk_layer(xp, mod_ctx, lp, ffn, rg_zero, dn_zero, False)
        xs, _, _ = trunk_layer(xs, mod_lat, lp, ffn, state_rglru[:, l], state_delta[:, l], True)
        rg_new.append(rg_s.astype(x_prompt.dtype))
        dn_new.append(dn_s.astype(x_prompt.dtype))
    y_prompt = rms_norm(xp, final_g)
    y_sample = rms_norm(xs, final_g)
    new_state_rglru = jnp.stack(rg_new, axis=1)
    new_state_delta = jnp.stack(dn_new, axis=1)
    return (y_prompt, y_sample, new_state_rglru, new_state_delta)
```

```python
import contextlib
import numpy as np
import concourse.bass as bass
import concourse.mybir as mybir
from concourse.bass_utils import run_bass_kernel_spmd

F32 = mybir.dt.float32
BF16 = mybir.dt.bfloat16
ALU = mybir.AluOpType
AF = mybir.ActivationFunctionType
AX = mybir.AxisListType

D = 2048
KC = 16
DEPTH = 2
NSEQ_P = 4
LP = 256
LS = 4096
NT = NSEQ_P * LP + LS
TT = 512
NTILE = NT // TT
NBLK = NT // 128
N_IN = 12816
OFF_GATE = 4624
D_FF = 5632
N_EXP = 8
D_FFE = 2816
EPS = 1e-6
SEQS = [(i * LP, LP, 0) for i in range(NSEQ_P)] + [(NSEQ_P * LP, LS, 1)]
NEG = -30000.0

ENGS = ("pe", "act", "dve", "pool", "sp")
N_DMA_SEMS = 4


class _Op:
    __slots__ = ("eng", "fn", "waits", "is_dma", "dsem", "dcount", "marked", "count")


class Prog:
    def __init__(self, nc):
        self.nc = nc
        self.ops = {e: [] for e in ENGS}
        self.res = {}
        self.dma_rr = {e: 0 for e in ENGS}
        self.dma_counts = {}

    def _add(self, eng, fn, reads, writes, is_dma=False, extra=()):
        op = _Op()
        op.eng, op.fn, op.is_dma, op.marked, op.count = eng, fn, is_dma, False, 0
        op.dsem, op.dcount = None, 0
        def _isps(k):
            return isinstance(k, str) and k.startswith("ps") and k[2:].isdigit()
        reads = [k[0] if isinstance(k, tuple) and _isps(k[0]) else k for k in reads]
        writes = [k[0] if isinstance(k, tuple) and _isps(k[0]) else k for k in writes]
        writes = writes + [k for k in reads if _isps(k)]
        reads = [k for k in reads if not _isps(k)]
        lst = self.ops[eng]
        idx = len(lst)
        deps = set(extra)
        for r in reads:
            st = self.res.get(r)
            if st is not None and st[0] is not None:
                deps.add(st[0])
        for w in writes:
            st = self.res.get(w)
            if st is not None:
                if st[0] is not None:
                    deps.add(st[0])
                for ei in st[1].values():
                    deps.add(ei)
        waits = []
        for (e, i) in deps:
            p = self.ops[e][i]
            if e == eng and not p.is_dma and not is_dma and eng == "pe":
                continue
            p.marked = True
            waits.append((e, i))
        op.waits = waits
        if is_dma:
            k = self.dma_rr[eng] % N_DMA_SEMS
            self.dma_rr[eng] += 1
            c = self.dma_counts.get((eng, k), 0) + 1
            self.dma_counts[(eng, k)] = c
            op.dsem, op.dcount = (eng, k), c
        lst.append(op)
        me = (eng, idx)
        rkey = (eng, op.dsem) if is_dma else eng
        for r in reads:
            st = self.res.setdefault(r, [None, {}])
            st[1][rkey] = me
        for w in writes:
            self.res[w] = [me, {}]
        return me

    def op(self, eng, fn, reads=(), writes=()):
        return self._add(eng, fn, reads, writes)

    def dma(self, eng, out, in_, reads=(), writes=()):
        return self._add(eng, lambda e: e.dma_start(out=out, in_=in_), reads, writes, is_dma=True)

    def barrier(self):
        last = []
        for e in ENGS:
            lst = self.ops[e]
            j = len(lst) - 1
            while j >= 0 and lst[j].is_dma:
                j -= 1
            if j >= 0 and lst[j].fn is not None:
                last.append((e, j))
            seen = set()
            for j in range(len(lst) - 1, -1, -1):
                if lst[j].is_dma and lst[j].dsem not in seen:
                    seen.add(lst[j].dsem)
                    last.append((e, j))
                if len(seen) == N_DMA_SEMS:
                    break
        for e in ENGS:
            self._add(e, None, (), (), extra=last)
        self.res = {}

    def emit(self):
        nc = self.nc
        with contextlib.ExitStack() as st:
            esem = {e: st.enter_context(nc.semaphore("s_" + e)) for e in ENGS}
            dsem = {}
            for key in sorted(self.dma_counts):
                dsem[key] = st.enter_context(nc.semaphore("d_%s%d" % key))
            for e in ENGS:
                c = 0
                for op in self.ops[e]:
                    if op.is_dma:
                        continue
                    if op.marked:
                        c += 1
                    op.count = c
            block = st.enter_context(nc.Block())

            def run(engname):
                def body(eng):
                    seen = {}
                    for op in self.ops[engname]:
                        for (pe_, pi) in op.waits:
                            p = self.ops[pe_][pi]
                            if p.is_dma:
                                key, val, sem = ("d",) + p.dsem, 16 * p.dcount, dsem[p.dsem]
                            else:
                                key, val, sem = ("e", pe_), p.count, esem[pe_]
                            if seen.get(key, 0) >= val:
                                continue
                            seen[key] = val
                            eng.wait_ge(sem, val)
                        if op.fn is None:
                            continue
                        ins = op.fn(eng)
                        if op.is_dma:
                            ins.then_inc(dsem[op.dsem], 16)
                        elif op.marked:
                            ins.then_inc(esem[engname], 1)
                    for (e, k), c in self.dma_counts.items():
                        if e == engname:
                            eng.wait_ge(dsem[(e, k)], 16 * c)
                return body

            block.tensor(run("pe"))
            block.scalar(run("act"))
            block.vector(run("dve"))
            block.gpsimd(run("pool"))
            block.sync(run("sp"))


class V:
    __slots__ = ("ap", "key")

    def __init__(self, ap, key):
        self.ap, self.key = ap, key


class Buf:
    def __init__(self, t, name, kax=None):
        self.t, self.name, self.kax = t, name, kax

    def __getitem__(self, idx):
        if not isinstance(idx, tuple):
            idx = (idx,)
        key = self.name
        if self.kax is not None and len(idx) > self.kax and isinstance(idx[self.kax], int):
            key = (self.name, idx[self.kax])
        return V(self.t[idx], key)

    def all_keys(self, n):
        return [(self.name, i) for i in range(n)]


class Rot:
    def __init__(self, bufs):
        self.bufs, self.i = bufs, 0

    def next(self):
        b = self.bufs[self.i % len(self.bufs)]
        self.i += 1
        return b


DECLARED = []
DBG_OUT = []


class _Stop(Exception):
    pass


def build(upto=None, dbg=()):
    nc = bass.Bass("TRN2", target_bir_lowering=False)
    P = Prog(nc)
    del DECLARED[:]
    del DBG_OUT[:]
    try:
        _build(nc, P, upto)
    except _Stop:
        pass
    for (name, shape, dt) in dbg:
        pass
    P.emit()
    return nc


def _build(nc, P, upto):
    step = [0]

    def checkpoint(tag, dumps=()):
        if upto is not None and tag == upto:
            for (name, v) in dumps:
                o = nc.dram_tensor("dbg_" + name, list(v.ap.shape), v.ap.dtype, kind="ExternalOutput").ap()
                DBG_OUT.append("dbg_" + name)
                P.dma("sp", o, v.ap, keys(v), [])
            raise _Stop()

    class Lazy:
        def __init__(self, name, shape):
            self.name, self.shape, self._ap = name, list(shape), None

        def ap(self):
            if self._ap is None:
                self._ap = nc.dram_tensor(self.name, self.shape, F32, kind="ExternalInput").ap()
                DECLARED.append(self.name)
            return self._ap

        def __getitem__(self, idx):
            return self.ap()[idx]

        def rearrange(self, *a, **k):
            return self.ap().rearrange(*a, **k)

        def partition_broadcast(self, n):
            return self.ap().partition_broadcast(n)

    def din(name, shape):
        return Lazy(name, shape)

    def dout(name, shape):
        return nc.dram_tensor(name, list(shape), F32, kind="ExternalOutput").ap()

    xin = din("xin", [NT, D])
    cT = din("cT", [128, KC, 2])
    consts = din("consts", [128, 9 * 128])
    icnt = din("icnt", [128, 4, LP + 64])
    ada_w = din("ada_w", [DEPTH, D, 6 * D])
    adab = din("adab", [DEPTH, 128, 96])
    ng = din("ng", [DEPTH, 128, 2, KC])
    w_in = din("w_in", [DEPTH, D, N_IN])
    rgv = din("rgv", [DEPTH, 128, 2, 4, 9])
    rg_wa = din("rg_wa", [DEPTH, 2, 4, 128, 128])
    rg_wx = din("rg_wx", [DEPTH, 2, 4, 128, 128])
    pool_w = din("pool_w", [DEPTH, 4, 128, 128])
    pool_sc = din("pool_sc", [DEPTH, 128, 4])
    sgu_rows = din("sgu_rows", [DEPTH, 3, 512])
    sgu_wsT = din("sgu_wsT", [DEPTH, 4, 128, 128])
    dn_cw = din("dn_cw", [DEPTH, 128, 2, 12, 4])
    dn_rows = din("dn_rows", [DEPTH, 3, 8])
    dn_ng = din("dn_ng", [DEPTH, 512])
    dn_s0 = din("dn_s0", [DEPTH, 2, 4, 128, 128])
    w_br = din("w_br", [DEPTH, 4, 512, D])
    w_out = din("w_out", [DEPTH, D, D])
    ffn_w1 = din("ffn_w1", [1, D, D_FF])
    ffn_w3 = din("ffn_w3", [1, D, D_FF])
    ffn_w2 = din("ffn_w2", [1, D_FF, D])
    moe_wr = din("moe_wr", [128, KC, N_EXP])
    moe_br = din("moe_br", [1, N_EXP])
    moe_w1 = din("moe_w1", [1, N_EXP, D, D_FFE])
    moe_w3 = din("moe_w3", [1, N_EXP, D, D_FFE])
    moe_w2 = din("moe_w2", [1, N_EXP, D_FFE, D])
    fin_g = din("fin_g", [128, KC])
    y_out = dout("y_out", [NT, D])
    rg_out = dout("rg_out", [NSEQ_P, DEPTH, 2, 512])
    dn_out = dout("dn_out", [NSEQ_P, DEPTH, 2, 4, 128, 128])
    def scr(name, shape, dt=F32):
        return Buf(nc.dram_tensor(name, list(shape), dt).ap(), name)

    XT = scr("XT", [128, KC, NT])
    HT = scr("HT", [128, KC, NT], BF16)
    FM = scr("FM", [128, 28, NT])
    TMV = scr("TMV", [NT, 512])
    TMZ = scr("TMZ", [NT, 512])
    TMB = scr("TMB", [NT, 16])
    QKN = [scr("QKN%d" % d, [128, 12, NT]) for d in range(2)]
    OSC = [scr("OSC%d" % d, [NT, 512]) for d in range(2)]
    YS = scr("YS", [128, KC, NT], BF16)

    def sb(name, shape, dt=F32, kax=None):
        return Buf(nc.alloc_sbuf_tensor(name, list(shape), dt), name, kax)

    cst = sb("cst", [128, 9, 128])
    IDENT, ONES, JREV, TRIF, TRIB, MBS_F, MBS_B, MBIT_F, MBIT_B = [cst[:, i, :] for i in range(9)]
    for v in (IDENT, ONES, JREV, TRIF, TRIB, MBS_F, MBS_B, MBIT_F, MBIT_B):
        v.key = "cst"
    csT = sb("csT", [128, KC, 2], BF16)
    modv = sb("modv", [128, 96, 2])
    MA1, MB1, MG1, MB2, MA2, MG2 = [sb("m%d" % i, [128, KC, 2]) for i in range(6)]
    ngs = sb("ngs", [128, 2, KC])
    fing = sb("fing", [128, KC])
    PS = [Buf(nc.alloc_psum_tensor("ps%d" % i, [128, 512], F32), "ps%d" % i) for i in range(8)]
    psr = Rot(PS)

    def psq():
        b = psr.next()
        return V(b.t[:, 0:128], b.name)

    def keys(*vs):
        out = []
        for v in vs:
            if isinstance(v, V):
                if isinstance(v.key, list):
                    out.extend(v.key)
                else:
                    out.append(v.key)
        return out

    def mm(out, lhsT, rhs, start=True, stop=True):
        P.op("pe", lambda e: e.matmul(out.ap, lhsT.ap, rhs.ap, start=start, stop=stop), keys(lhsT, rhs), keys(out))

    def tr(out, in_):
        P.op("pe", lambda e: e.transpose(out.ap, in_.ap, IDENT.ap), keys(in_, IDENT), keys(out))

    def act(out, in_, func, bias=None, scale=1.0, accum=None, eng="act"):
        kw = {}
        if bias is not None:
            kw["bias"] = bias.ap if isinstance(bias, V) else bias
        kw["scale"] = scale.ap if isinstance(scale, V) else scale
        if accum is not None:
            kw["accum_out"] = accum.ap
        P.op("act", lambda e: e.activation(out.ap, in_.ap, func, **kw), keys(in_, bias, scale), keys(out, accum))

    def tt(out, a, b, op, eng="dve"):
        P.op(eng, lambda e: e.tensor_tensor(out.ap, a.ap, b.ap, op), keys(a, b), keys(out))

    def ts(out, a, s1, op0, s2=None, op1=None, eng="dve"):
        s1a = s1.ap if isinstance(s1, V) else s1
        s2a = s2.ap if isinstance(s2, V) else s2
        if op1 is None:
            P.op(eng, lambda e: e.tensor_scalar(out.ap, a.ap, s1a, None, op0), keys(a, s1), keys(out))
        else:
            P.op(eng, lambda e: e.tensor_scalar(out.ap, a.ap, s1a, s2a, op0, op1), keys(a, s1, s2), keys(out))

    def stt(out, a, s, b, op0, op1, eng="dve"):
        sa = s.ap if isinstance(s, V) else s
        P.op(eng, lambda e: e.scalar_tensor_tensor(out.ap, a.ap, sa, b.ap, op0, op1), keys(a, s, b), keys(out))

    def rsq(out, in_, scale, eps):
        act(out, in_, AF.Sqrt, bias=eps, scale=scale)
        P.op("dve", lambda e: e.reciprocal(out.ap, out.ap), keys(out), keys(out))

    def cp(out, in_, eng="dve"):
        P.op(eng, lambda e: e.tensor_copy(out.ap, in_.ap), keys(in_), keys(out))

    def memset(v, val, eng="pool"):
        P.op(eng, lambda e: e.memset(v.ap, val), (), keys(v))

    def ld(out, in_, eng="sp"):
        if isinstance(in_, Lazy):
            in_ = in_.ap()
        P.dma(eng, out.ap, in_.ap if isinstance(in_, V) else in_, keys(in_), keys(out))

    def st(out, in_, eng="sp"):
        P.dma(eng, out.ap if isinstance(out, V) else out, in_.ap, keys(in_), keys(out))

    class Phase:
        n_ph = [0]

        def __enter__(self):
            self.st = contextlib.ExitStack()
            Phase.n_ph[0] += 1
            self.id = Phase.n_ph[0]
            return self

        def sb(self, name, shape, dt=F32, kax=None):
            name = "%s_p%d" % (name, self.id)
            t = self.st.enter_context(nc.sbuf_tensor(name, list(shape), dt))
            return Buf(t, name, kax)

        def __exit__(self, *a):
            P.barrier()
            self.st.close()
            return False

    ld(cst[:, :, :], consts.rearrange("p (a b) -> p a b", a=9))
    ld(fing[:, :], fin_g)
    with Phase() as ph:
        ctf = ph.sb("ctf", [128, KC, 2])
        ld(ctf[:, :, :], cT)
        act(csT[:, :, :], ctf[:, :, :], AF.Silu)

    with Phase() as ph:
        xa = [ph.sb("xa%d" % i, [128, D]) for i in range(2)]
        xtb = [ph.sb("xtb%d" % i, [128, KC, 128]) for i in range(2)]
        for tb in range(NBLK):
            a, o = xa[tb % 2], xtb[tb % 2]
            ld(a[:, :], xin[tb * 128:(tb + 1) * 128, :], "sp" if tb % 2 == 0 else "act")
            for g in range(4):
                ps = psr.next()
                for j in range(4):
                    tr(V(ps.t[:, j * 128:(j + 1) * 128], ps.name), a[:, (4 * g + j) * 128:(4 * g + j + 1) * 128])
                cp(V(o.t[:, 4 * g:4 * g + 4, :], o.name), V(ps.t[:, :].rearrange("p (a b) -> p a b", a=4), ps.name),
                   "dve")
            st(XT[:, :, tb * 128:(tb + 1) * 128], o[:, :, :])

    checkpoint("p0", [("XT", XT[:, :, :])])

    def wslice(wap, c0, c1, r0=0, nk=KC):
        return wap[r0:r0 + nk * 128, c0:c1].rearrange("(k p) n -> p k n", p=128)

    def rms_tile(ph_bufs, xt, t, MA, MB, grp, hT, hf=None):
        sq, rstd, tmp = ph_bufs
        ps = psr.next()
        for kc in range(KC):
            s = sq.next()
            act(s[:, :], xt[:, kc, :], AF.Square)
            mm(ps[:, :], ONES, s[:, :], start=(kc == 0), stop=(kc == KC - 1))
        rsq(rstd[:, :], ps[:, :], 1.0 / D, EPS)
        for kc in range(KC):
            m = tmp.next()
            tt(m[:, :], xt[:, kc, :], rstd[:, :], ALU.mult)
            if hf is not None:
                act(hf[:, kc, :], m[:, :], AF.Identity, bias=MB[:, kc, grp:grp + 1], scale=MA[:, kc, grp:grp + 1])
                cp(hT[:, kc, :], hf[:, kc, :], "pool")
            else:
                act(hT[:, kc, :], m[:, :], AF.Identity, bias=MB[:, kc, grp:grp + 1], scale=MA[:, kc, grp:grp + 1])

    def gelu_from(ph_b, out, ps, n):
        x, t = ph_b
        act(x[:, 0:n], ps, AF.Identity)
        act(t[:, 0:n], ps, AF.Square)
        ts(t[:, 0:n], t[:, 0:n], 0.044715, ALU.mult, 1.0, ALU.add)
        tt(t[:, 0:n], t[:, 0:n], x[:, 0:n], ALU.mult)
        act(t[:, 0:n], t[:, 0:n], AF.Sigmoid, scale=1.5957691216057308)
        tt(out, t[:, 0:n], x[:, 0:n], ALU.mult)

    for l in range(DEPTH):
        with Phase() as ph:
            was = Rot([ph.sb("wa%d" % i, [128, KC, 512], BF16) for i in range(2)])
            abt = ph.sb("abt", [128, 96])
            ld(abt[:, :], adab[l])
            ld(ngs[:, :, :], ng[l])
            psm = PS[7]
            for g in range(24):
                wa = was.next()
                ld(wa[:, :, :], wslice(ada_w[l], g * 512, (g + 1) * 512), "pool")
                for j in range(4):
                    n = g * 4 + j
                    for kc in range(KC):
                        mm(V(psm.t[:, 2 * n:2 * n + 2], psm.name), wa[:, kc, j * 128:(j + 1) * 128], csT[:, kc, :],
                           start=(kc == 0), stop=(kc == KC - 1))
            ps3 = psm.t[:, 0:192].rearrange("p (a b) -> p a b", b=2)
            for i in range(2):
                tt(modv[:, :, i], V(ps3[:, :, i], psm.name), abt[:, :], ALU.add)
            for i in range(2):
                cp(MB1[:, :, i], modv[:, 0:16, i])
                stt(MA1[:, :, i], modv[:, 16:32, i], 1.0, ngs[:, 0, :], ALU.add, ALU.mult)
                cp(MG1[:, :, i], modv[:, 32:48, i])
                cp(MB2[:, :, i], modv[:, 48:64, i])
                stt(MA2[:, :, i], modv[:, 64:80, i], 1.0, ngs[:, 1, :], ALU.add, ALU.mult)
                cp(MG2[:, :, i], modv[:, 80:96, i])

        checkpoint("mod%d" % l, [("modv", modv[:, :, :]), ("MA1", MA1[:, :, :])])
        with Phase() as ph:
            xt = ph.sb("xt", [128, KC, TT], F32, 1)
            hT = ph.sb("hT", [128, KC, TT], BF16, 1)
            sq = Rot([ph.sb("sq%d" % i, [128, TT]) for i in range(2)])
            tmp = Rot([ph.sb("tmp%d" % i, [128, TT]) for i in range(2)])
            rstd = ph.sb("rstd", [128, TT])
            wts = Rot([ph.sb("w%d" % i, [128, KC, 512], BF16) for i in range(3)])
            stg = Rot([ph.sb("stg%d" % i, [128, TT]) for i in range(3)])
            gx = ph.sb("gx", [128, TT])
            gt = ph.sb("gt", [128, TT])
            for t in range(NTILE):
                t0 = t * TT
                grp = 0 if t0 < NSEQ_P * LP else 1
                for kc in range(KC):
                    ld(xt[:, kc, :], XT[:, kc, t0:t0 + TT], "sp" if kc % 2 == 0 else "act")
                rms_tile((sq, rstd, tmp), xt, t, MA1, MB1, grp, hT)
                for kc in range(KC):
                    st(HT[:, kc, t0:t0 + TT], hT[:, kc, :])
                for (c0, fb, gel) in ((0, 0, 0), (512, 4, 1), (1024, 8, 0), (1536, 12, 1), (2560, 16, 0),
                                      (3072, 20, 0), (3584, 24, 0)):
                    w = wts.next()
                    ld(w[:, :, :], wslice(w_in[l], c0, c0 + 512), "pool")
                    for j in range(4):
                        ps = psr.next()
                        for kc in range(KC):
                            mm(ps[:, :], w[:, kc, j * 128:(j + 1) * 128], hT[:, kc, :], start=(kc == 0),
                               stop=(kc == KC - 1))
                        s = stg.next()
                        if gel:
                            gelu_from((gx, gt), s[:, :], ps[:, :], TT)
                        else:
                            act(s[:, :], ps[:, :], AF.Identity)
                        st(FM[:, fb + j, t0:t0 + TT], s[:, :])
                for (c0, ncol, dst, gel) in ((2048, 512, TMV, 1), (4096, 512, TMZ, 0), (4608, 16, TMB, 0)):
                    w = wts.next()
                    ld(V(w.t[:, :, 0:ncol], w.name), wslice(w_in[l], c0, c0 + ncol), "pool")
                    for tb in range(TT // 128):
                        ps = psr.next()
                        for kc in range(KC):
                            mm(V(ps.t[:, 0:ncol], ps.name), hT[:, kc, tb * 128:(tb + 1) * 128],
                               V(w.t[:, kc, 0:ncol], w.name), start=(kc == 0), stop=(kc == KC - 1))
                        s = stg.next()
                        if gel:
                            gelu_from((gx, gt), V(s.t[:, 0:ncol], s.name), V(ps.t[:, 0:ncol], ps.name), ncol)
                        else:
                            act(V(s.t[:, 0:ncol], s.name), V(ps.t[:, 0:ncol], ps.name), AF.Identity)
                        st(V(dst.t[t0 + tb * 128:t0 + (tb + 1) * 128, :], dst.name), V(s.t[:, 0:ncol], s.name))

        checkpoint("ph1_%d" % l, [("HT", HT[:, :, :]), ("FM", FM[:, :, :]), ("TMV", TMV[:, :]), ("TMZ", TMZ[:, :]), ("TMB", TMB[:, :])])
        with Phase() as ph:
            LM = LS
            xr = ph.sb("xr", [128, LM])
            xv = ph.sb("xv", [128, LM])
            xc = ph.sb("xc", [128, LM])
            rr = ph.sb("rr", [128, LM])
            ii = ph.sb("ii", [128, LM])
            hf = ph.sb("hf", [128, LM])
            hb = ph.sb("hb", [128, LM])
            yb16 = ph.sb("yb16", [128, LM], BF16)
            rt = Rot([ph.sb("rt%d" % i, [128, 128]) for i in range(2)])
            rv = ph.sb("rv", [128, 2, 4, 9])
            wab = ph.sb("wab", [128, 2, 4, 128])
            wxb = ph.sb("wxb", [128, 2, 4, 128])
            c1 = ph.sb("c1", [128, 2, 4])
            zero1 = ph.sb("zero1", [128, 1])
            memset(zero1[:, :], 0.0)
            ld(rv[:, :, :, :], rgv[l])
            ld(wab[:, :, :, :], rg_wa[l].rearrange("d h i j -> i d h j"))
            ld(wxb[:, :, :, :], rg_wx[l].rearrange("d h i j -> i d h j"))
            act(c1[:, :, :], rv[:, :, :, 7], AF.Exp, scale=-1.0)
            act(c1[:, :, :], c1[:, :, :], AF.Ln, bias=1.0)
            ts(c1[:, :, :], c1[:, :, :], -8.0, ALU.mult)

            def reverse(dst, src, L):
                nb = L // 128
                for b in range(nb):
                    p1 = psq()
                    tr(p1, V(src.t[:, b * 128:(b + 1) * 128], src.name))
                    r = rt.next()
                    cp(r[:, :], p1, "dve")
                    p2 = psq()
                    mm(p2, r[:, :], JREV)
                    act(V(dst.t[:, (nb - 1 - b) * 128:(nb - b) * 128], dst.name), p2, AF.Identity)

            def rg_dir(src, dst, L, d, h, h0):
                sc = lambda k: rv[:, d, h, k:k + 1]
                X = lambda a, b: V(src.t[:, a:b], src.name)
                C = lambda a, b: V(xc.t[:, a:b], xc.name)
                ts(C(0, L), X(0, L), sc(3), ALU.mult, sc(4), ALU.add)
                for k in (1, 2, 3):
                    stt(C(k, L), X(0, L - k), sc(3 - k), C(k, L), ALU.mult, ALU.add)
                for t0 in range(0, L, TT):
                    n = min(TT, L - t0)
                    ps = psr.next()
                    mm(V(ps.t[:, 0:n], ps.name), wab[:, d, h, :], C(t0, t0 + n))
                    act(V(rr.t[:, t0:t0 + n], rr.name), V(ps.t[:, 0:n], ps.name), AF.Sigmoid, bias=sc(5))
                    ps = psr.next()
                    mm(V(ps.t[:, 0:n], ps.name), wxb[:, d, h, :], C(t0, t0 + n))
                    act(V(ii.t[:, t0:t0 + n], ii.name), V(ps.t[:, 0:n], ps.name), AF.Sigmoid, bias=sc(6))
                R = V(rr.t[:, 0:L], rr.name)
                I = V(ii.t[:, 0:L], ii.name)
                act(R, R, AF.Exp, scale=c1[:, d, h:h + 1])
                tt(I, I, C(0, L), ALU.mult)
                tt(C(0, L), R, R, ALU.mult)
                act(C(0, L), C(0, L), AF.Sqrt, bias=1.0, scale=-1.0)
                tt(I, I, C(0, L), ALU.mult)
                h0a = h0.ap
                P.op("dve", lambda e: e.tensor_tensor_scan(dst.t[:, 0:L], rr.t[:, 0:L], ii.t[:, 0:L], h0a, ALU.mult,
                                                            ALU.add), [rr.name, ii.name, h0.key], [dst.name])

            for si, (s0, L, smp) in enumerate(SEQS):
                for h in range(4):
                    ld(V(xr.t[:, 0:L], xr.name), FM[:, h, s0:s0 + L])
                    h0f = rv[:, 0, h, 8:9] if smp else zero1[:, :]
                    h0b = rv[:, 1, h, 8:9] if smp else zero1[:, :]
                    rg_dir(xr, hf, L, 0, h, h0f)
                    reverse(xv, xr, L)
                    rg_dir(xv, hb, L, 1, h, h0b)
                    if not smp:
                        st(rg_out[si, l, 0, h * 128:(h + 1) * 128].rearrange("(p o) -> p o", o=1),
                           V(hf.t[:, L - 1:L], hf.name))
                        st(rg_out[si, l, 1, h * 128:(h + 1) * 128].rearrange("(p o) -> p o", o=1),
                           V(hb.t[:, L - 1:L], hb.name))
                    reverse(xv, hb, L)
                    tt(V(hf.t[:, 0:L], hf.name), V(hf.t[:, 0:L], hf.name), V(xv.t[:, 0:L], xv.name), ALU.add)
                    ld(V(xr.t[:, 0:L], xr.name), FM[:, 4 + h, s0:s0 + L])
                    tt(V(yb16.t[:, 0:L], yb16.name), V(hf.t[:, 0:L], hf.name), V(xr.t[:, 0:L], xr.name), ALU.mult)
                    st(YS[:, h, s0:s0 + L], V(yb16.t[:, 0:L], yb16.name))

        checkpoint("rg%d" % l, [("YS", YS[:, :, :])])
        with Phase() as ph:
            ic = ph.sb("ic", [128, 4, LP + 64])
            ld(ic[:, :, :], icnt)
            pw = ph.sb("pw", [128, 4, 128])
            ld(pw[:, :, :], pool_w[l].rearrange("g c d -> c g d"))
            psc = ph.sb("psc", [128, 4])
            ld(psc[:, :], pool_sc[l])
            WP, WS = LP + 16, 64 + 16
            pbuf = [ph.sb("pb%d" % i, [128, max(2 * WP, 8 * WS)]) for i in range(5)]
            pl = Rot([ph.sb("pl%d" % i, [128, TT]) for i in range(2)])
            py = Rot([ph.sb("py%d" % i, [128, TT], BF16) for i in range(2)])
            for b_ in pbuf:
                memset(b_[:, :], 0.0)
            for t in range(NTILE):
                t0 = t * TT
                smp = t0 >= NSEQ_P * LP
                R, W = (64, WS) if smp else (LP, WP)
                nr = TT // R
                if t0 == NSEQ_P * LP:
                    memset(pbuf[0][:, :], 0.0)
                for j in range(4):
                    v3 = lambda k: pbuf[k].t[:, 0:nr * W].rearrange("p (r w) -> p r w", w=W)
                    xin_v = V(v3(0)[:, :, 8:8 + R], pbuf[0].name)
                    ld(xin_v, V(FM.t[:, 8 + j, t0:t0 + TT].rearrange("p (r w) -> p r w", w=R), FM.name))
                    sh = [1, 1, 2, 4]
                    lo = [1, 2, 4, 8]
                    for lev in range(j + 1):
                        src = v3(lev)
                        dstv = v3(lev + 1)
                        a, s = lo[lev], sh[lev]
                        n = W - 2 * a + 1 if lev > 0 else W - 1
                        if lev == 0:
                            tt(V(dstv[:, :, 1:W], pbuf[1].name), V(src[:, :, 0:W - 1], pbuf[0].name),
                               V(src[:, :, 1:W], pbuf[0].name), ALU.add)
                        else:
                            tt(V(dstv[:, :, a:W - a + 1], pbuf[lev + 1].name),
                               V(src[:, :, a - s:W - a + 1 - s], pbuf[lev].name),
                               V(src[:, :, a + s:W - a + 1 + s], pbuf[lev].name), ALU.add)
                    p_ = pl.next()
                    p3 = p_.t[:, :].rearrange("p (r w) -> p r w", w=R)
                    ioff = LP if smp else 0
                    icv = V(ic.t[:, j, ioff:ioff + R].unsqueeze(1).broadcast_to([128, nr, R]), ic.name)
                    tt(V(p3, p_.name), V(v3(j + 1)[:, :, 8:8 + R], pbuf[j + 1].name), icv, ALU.mult)
                    tt(V(p3, p_.name), V(p3, p_.name), xin_v, ALU.subtract)
                    ps = psr.next()
                    mm(ps[:, :], pw[:, j, :], p_[:, :])
                    y = py.next()
                    act(y[:, :], ps[:, :], AF.Identity, scale=psc[:, j:j + 1])
                    st(YS[:, 4 + j, t0:t0 + TT], y[:, :])

        checkpoint("pool%d" % l, [("YS", YS[:, :, :])])
        with Phase() as ph:
            rows = ph.sb("rows", [128, 3, 512])
            ld(rows[:, :, :], sgu_rows[l].partition_broadcast(128))
            wsT = ph.sb("wsT", [128, 4, 128])
            ld(wsT[:, :, :], sgu_wsT[l].rearrange("h p q -> p h q"))
            vt = Rot([ph.sb("vt%d" % i, [128, 512]) for i in range(2)])
            ut = Rot([ph.sb("ut%d" % i, [128, 4, 128]) for i in range(2)])
            cen = ph.sb("cen", [128, 512])
            sqv = ph.sb("sqv", [128, 512])
            st1 = Rot([ph.sb("st1%d" % i, [128, 2]) for i in range(2)])
            sy = Rot([ph.sb("sy%d" % i, [128, 4, 128]) for i in range(2)])
            syb = Rot([ph.sb("syb%d" % i, [128, 4, 128], BF16) for i in range(2)])
            for b in range(NBLK):
                b0 = b * 128
                v = vt.next()
                u = ut.next()
                ld(v[:, :], V(TMV.t[b0:b0 + 128, :], TMV.name))
                ld(u[:, :, :], FM[:, 12:16, b0:b0 + 128], "act")
                s1 = st1.next()
                P.op("dve", lambda e, s1=s1, v=v: e.tensor_reduce(s1.t[:, 0:1], v.t[:, :], AX.X, ALU.add),
                     [v.name], [s1.name])
                ts(s1[:, 0:1], s1[:, 0:1], -1.0 / 512, ALU.mult)
                act(cen[:, :], v[:, :], AF.Identity, bias=s1[:, 0:1])
                act(sqv[:, :], cen[:, :], AF.Square, accum=s1[:, 1:2])
                rsq(s1[:, 1:2], s1[:, 1:2], 1.0 / 512, EPS)
                stt(cen[:, :], cen[:, :], s1[:, 1:2], rows[:, 0, :], ALU.mult, ALU.mult)
                tt(cen[:, :], cen[:, :], rows[:, 1, :], ALU.add)
                y = sy.next()
                yb = syb.next()
                for h in range(4):
                    p = psq()
                    mm(p, cen[:, h * 128:(h + 1) * 128], wsT[:, h, :])
                    tt(y[:, h, :], p, rows[:, 2, h * 128:(h + 1) * 128], ALU.add)
                tt(yb[:, :, :], y[:, :, :], u[:, :, :], ALU.mult)
                st(YS[:, 8:12, b0:b0 + 128], yb[:, :, :])

        checkpoint("sgu%d" % l, [("YS", YS[:, :, :])])
        with Phase() as ph:
            cw = ph.sb("cw", [128, 2, 12, 4])
            ld(cw[:, :, :, :], dn_cw[l])
            xb = Rot([ph.sb("xb%d" % i, [128, TT + 6]) for i in range(2)])
            cv = Rot([ph.sb("cv%d" % i, [128, TT]) for i in range(2)])
            sg = Rot([ph.sb("sg%d" % i, [128, TT]) for i in range(2)])
            sq2 = Rot([ph.sb("sq2%d" % i, [128, TT]) for i in range(2)])
            rs = Rot([ph.sb("rs%d" % i, [128, TT]) for i in range(2)])
            for x_ in xb.bufs:
                memset(x_[:, :], 0.0)
            for (s0, L, smp) in SEQS:
                CT = min(TT, L)
                for t0 in range(s0, s0 + L, CT):
                    for c in range(12):
                        x_ = xb.next()
                        lo_ = max(s0, t0 - 3)
                        hi_ = min(s0 + L, t0 + CT + 3)
                        if lo_ > t0 - 3:
                            memset(V(x_.t[:, 0:3], x_.name), 0.0)
                        if hi_ < t0 + CT + 3:
                            memset(V(x_.t[:, CT + 3:CT + 6], x_.name), 0.0)
                        ld(V(x_.t[:, 3 - (t0 - lo_):3 + (hi_ - t0)], x_.name), FM[:, 16 + c, lo_:hi_],
                           "sp" if c % 2 == 0 else "act")
                        for d in range(2):
                            o = cv.next()
                            O = V(o.t[:, 0:CT], o.name)
                            xs = lambda k: V(x_.t[:, 3 + k:3 + k + CT], x_.name)
                            wj = lambda j: cw[:, d, c, j:j + 1]
                            sgn = -1 if d == 0 else 1
                            ts(O, xs(0), wj(3), ALU.mult)
                            for k in (1, 2, 3):
                                stt(O, xs(sgn * k), wj(3 - k), O, ALU.mult, ALU.add)
                            s_ = sg.next()
                            S_ = V(s_.t[:, 0:CT], s_.name)
                            act(S_, O, AF.Silu)
                            if c < 8:
                                q_ = sq2.next()
                                Q_ = V(q_.t[:, 0:CT], q_.name)
                                act(Q_, S_, AF.Square)
                                ps = psr.next()
                                PSV = V(ps.t[:, 0:CT], ps.name)
                                mm(PSV, ONES, Q_)
                                r_ = rs.next()
                                R_ = V(r_.t[:, 0:CT], r_.name)
                                rsq(R_, PSV, 1.0, EPS)
                                if c < 4:
                                    stt(S_, S_, 128.0 ** -0.5, R_, ALU.mult, ALU.mult)
                                else:
                                    tt(S_, S_, R_, ALU.mult)
                            st(QKN[d][:, c, t0:t0 + CT], S_)

        checkpoint("dn1_%d" % l, [("QKN0", QKN[0][:, :, :]), ("QKN1", QKN[1][:, :, :])])
        with Phase() as ph:
            bat = ph.sb("bat", [128, NBLK, 16])
            ld(bat[:, :, :], V(TMB.t[:, :].rearrange("(b p) c -> p b c", p=128), TMB.name))
            drow = ph.sb("drow", [128, 3, 8])
            ld(drow[:, :, :], dn_rows[l].partition_broadcast(128))
            beta = ph.sb("beta", [128, NBLK, 8])
            gg = ph.sb("gg", [128, NBLK, 8])
            t8 = ph.sb("t8", [128, NBLK, 8])
            ea = ph.sb("ea", [128, 8])
            for d in range(2):
                act(beta[:, :, d * 4:d * 4 + 4], bat[:, :, d * 8:d * 8 + 4], AF.Sigmoid)
                tt(gg[:, :, d * 4:d * 4 + 4], bat[:, :, d * 8 + 4:d * 8 + 8],
                   V(drow.t[:, 1, d * 4:d * 4 + 4].unsqueeze(1).broadcast_to([128, NBLK, 4]), drow.name), ALU.add)
            stt(t8[:, :, :], gg[:, :, :], -1.0, gg[:, :, :], ALU.mult, ALU.max)
            act(t8[:, :, :], t8[:, :, :], AF.Exp, scale=-1.0)
            act(t8[:, :, :], t8[:, :, :], AF.Ln, bias=1.0)
            stt(gg[:, :, :], gg[:, :, :], 0.0, t8[:, :, :], ALU.max, ALU.add)
            act(ea[:, :], drow[:, 0, :], AF.Exp)
            ts(ea[:, :], ea[:, :], -1.0, ALU.mult)
            tt(gg[:, :, :], gg[:, :, :], V(ea.t[:, :].unsqueeze(1).broadcast_to([128, NBLK, 8]), ea.name), ALU.mult)

            NI = 8
            S = [ph.sb("S%d" % i, [128, 128]) for i in range(NI)]
            qkv_t = [ph.sb("qkv%d" % i, [128, 3, 128]) for i in range(NI)]
            gcol = [ph.sb("gcol%d" % i, [128, 8]) for i in range(2)]
            sc5 = [ph.sb("sc5%d" % i, [128, 5]) for i in range(NI)]
            gbc = [ph.sb("gbc%d" % i, [128, 128]) for i in range(NI)]
            argn = [ph.sb("argn%d" % i, [128, 128]) for i in range(NI)]
            argq = [ph.sb("argq%d" % i, [128, 128]) for i in range(NI)]
            egr = [ph.sb("egr%d" % i, [128, 128]) for i in range(NI)]
            Nm = [[ph.sb("N%d_%d" % (i, k), [128, 128]) for k in range(2)] for i in range(NI)]
            Xm = [[ph.sb("X%d_%d" % (i, k), [128, 128]) for k in range(2)] for i in range(NI)]
            Tm = [[ph.sb("T%d_%d" % (i, k), [128, 128]) for k in range(2)] for i in range(NI)]
            Rm = [ph.sb("R%d" % i, [128, 256]) for i in range(NI)]
            kend = [ph.sb("kend%d" % i, [128, 128]) for i in range(NI)]
            val = [ph.sb("val%d" % i, [128, 128]) for i in range(NI)]
            kcT = [ph.sb("kcT%d" % i, [128, 128]) for i in range(NI)]
            qgT = [ph.sb("qgT%d" % i, [128, 128]) for i in range(NI)]
            qkm = [ph.sb("qkm%d" % i, [128, 128]) for i in range(NI)]
            uu = [ph.sb("uu%d" % i, [128, 128]) for i in range(NI)]
            oo = [ph.sb("oo%d" % i, [128, 128]) for i in range(NI)]

            for si, (s0, L, smp) in enumerate(SEQS):
                nch = L // 128
                for i in range(NI):
                    d, h = i // 4, i % 4
                    if smp:
                        ld(S[i][:, :], dn_s0[l, d, h], "sp" if i % 2 == 0 else "act")
                    else:
                        memset(S[i][:, :], 0.0)
                for step in range(nch):
                    for d in range(2):
                        ch = step if d == 0 else nch - 1 - step
                        blk = (s0 + ch * 128) // 128
                        TRI = TRIF if d == 0 else TRIB
                        p = psq()
                        mm(V(p.ap[:, 0:4], p.key), TRI, gg[:, blk, d * 4:d * 4 + 4])
                        mm(V(p.ap[:, 4:8], p.key), ONES, gg[:, blk, d * 4:d * 4 + 4])
                        cp(gcol[d][:, :], V(p.ap[:, 0:8], p.key))
                    for i in range(NI):
                        d, h = i // 4, i % 4
                        ch = step if d == 0 else nch - 1 - step
                        tk0 = s0 + ch * 128
                        blk = tk0 // 128
                        TRI = TRIF if d == 0 else TRIB
                        MBS = MBS_F if d == 0 else MBS_B
                        MBIT = MBIT_F if d == 0 else MBIT_B
                        hcol = d * 4 + h
                        bcol = beta[:, blk, hcol:hcol + 1]
                        gc = gcol[d][:, h:h + 1]
                        gtot = gcol[d][:, 4 + h:5 + h]
                        q3 = qkv_t[i]
                        for k in range(3):
                            ld(q3[:, k, :], QKN[d][:, 4 * k + h, tk0:tk0 + 128], "sp" if (i + k) % 2 == 0 else "act")
                        qT, kT, vT = q3[:, 0, :], q3[:, 1, :], q3[:, 2, :]
                        s5 = sc5[i]
                        ts(s5[:, 0:1], gc, -1.0, ALU.mult)
                        act(s5[:, 4:5], gc, AF.Exp)
                        tt(s5[:, 1:2], s5[:, 4:5], bcol, ALU.mult)
                        act(s5[:, 2:3], gc, AF.Exp, bias=gtot, scale=-1.0)
                        act(s5[:, 3:4], gtot, AF.Exp)
                        ts(gbc[i][:, :], ONES, gg[:, blk, hcol:hcol + 1], ALU.mult)
                        pg = psq()
                        mm(pg, gbc[i][:, :], TRI)
                        stt(argn[i][:, :], pg, -1.0, MBS, ALU.mult, ALU.add)
                        tt(argq[i][:, :], pg, MBIT, ALU.add)
                        act(egr[i][:, :], pg, AF.Exp)
                        act(argn[i][:, :], argn[i][:, :], AF.Exp, bias=gc)
                        act(argq[i][:, :], argq[i][:, :], AF.Exp, bias=s5[:, 0:1])
                        pk = psq()
                        mm(pk, kT, kT)
                        N0, X0, T0 = Nm[i][0], Xm[i][0], Tm[i][0]
                        stt(N0[:, :], pk, bcol, argn[i][:, :], ALU.mult, ALU.mult)
                        px = psq()
                        tr(px, N0[:, :])
                        cp(X0[:, :], px, "dve")
                        tt(T0[:, :], IDENT, X0[:, :], ALU.subtract)
                        pq = psq()
                        mm(pq, kT, qT)
                        tt(qkm[i][:, :], pq, argq[i][:, :], ALU.mult)
                        tt(qgT[i][:, :], qT, egr[i][:, :], ALU.mult, eng="pool")
                        pkt = psq()
                        tr(pkt, kT)
                        ts(V(Rm[i].t[:, 128:256], Rm[i].name), pkt, s5[:, 1:2], ALU.mult)
                        act(kend[i][:, :], pkt, AF.Identity, scale=s5[:, 2:3])
                        pvt = psq()
                        tr(pvt, vT)
                        ts(V(Rm[i].t[:, 0:128], Rm[i].name), pvt, bcol, ALU.mult)
                    cur = 0
                    for lev in range(1, 7):
                        nxt = 1 - cur
                        for i in range(NI):
                            Nc, Xc, Tc = Nm[i][cur], Xm[i][cur], Tm[i][cur]
                            Nn, Xn, Tn = Nm[i][nxt], Xm[i][nxt], Tm[i][nxt]
                            pn = psq()
                            mm(pn, Xc[:, :], Nc[:, :])
                            act(Nn[:, :], pn, AF.Identity)
                            if lev < 6:
                                pxx = psq()
                                mm(pxx, Nc[:, :], Xc[:, :])
                                cp(Xn[:, :], pxx, "dve")
                            pt = psq()
                            mm(pt, Nn[:, :], Tc[:, :])
                            tt(Tn[:, :], pt, Tc[:, :], ALU.add)
                        cur = nxt
                    for i in range(NI):
                        d, h = i // 4, i % 4
                        ch = step if d == 0 else nch - 1 - step
                        tk0 = s0 + ch * 128
                        Tf = Tm[i][cur]
                        s5 = sc5[i]
                        pw_ = psr.next()
                        mm(V(pw_.t[:, 0:128], pw_.name), Tf[:, :], V(Rm[i].t[:, 0:128], Rm[i].name))
                        cp(val[i][:, :], V(pw_.t[:, 0:128], pw_.name), "dve")
                        pkc = psq()
                        mm(pkc, V(Rm[i].t[:, 128:256], Rm[i].name), Tf[:, :])
                        act(kcT[i][:, :], pkc, AF.Identity)
                        pu = psq()
                        mm(pu, kcT[i][:, :], S[i][:, :])
                        tt(uu[i][:, :], val[i][:, :], pu, ALU.subtract)
                        po = psq()
                        mm(po, qgT[i][:, :], S[i][:, :], start=True, stop=False)
                        mm(po, qkm[i][:, :], uu[i][:, :], start=False, stop=True)
                        act(oo[i][:, :], po, AF.Identity)
                        st(V(OSC[d].t[tk0:tk0 + 128, h * 128:(h + 1) * 128], OSC[d].name), oo[i][:, :],
                           "sp" if i % 2 == 0 else "act")
                        pS = psq()
                        mm(pS, kend[i][:, :], uu[i][:, :])
                        stt(S[i][:, :], S[i][:, :], s5[:, 3:4], pS, ALU.mult, ALU.add)
                if not smp:
                    for i in range(NI):
                        d, h = i // 4, i % 4
                        st(dn_out[si, l, d, h], S[i][:, :])

        checkpoint("dn2_%d" % l, [("OSC0", OSC[0][:, :]), ("OSC1", OSC[1][:, :])])
        with Phase() as ph:
            gro = ph.sb("gro", [128, 512])
            ld(gro[:, :], dn_ng[l].partition_broadcast(128))
            o0 = Rot([ph.sb("o0%d" % i, [128, 512]) for i in range(2)])
            o1 = Rot([ph.sb("o1%d" % i, [128, 512]) for i in range(2)])
            zz = Rot([ph.sb("zz%d" % i, [128, 512]) for i in range(2)])
            sqd = ph.sb("sqd", [128, 512])
            s4 = Rot([ph.sb("s4%d" % i, [128, 4]) for i in range(2)])
            yd = Rot([ph.sb("yd%d" % i, [128, 4, 128], BF16) for i in range(2)])
            for b in range(NBLK):
                b0 = b * 128
                a, b1, z = o0.next(), o1.next(), zz.next()
                ld(a[:, :], V(OSC[0].t[b0:b0 + 128, :], OSC[0].name))
                ld(b1[:, :], V(OSC[1].t[b0:b0 + 128, :], OSC[1].name), "act")
                ld(z[:, :], V(TMZ.t[b0:b0 + 128, :], TMZ.name))
                tt(a[:, :], a[:, :], b1[:, :], ALU.add)
                s = s4.next()
                for h in range(4):
                    act(sqd[:, h * 128:(h + 1) * 128], a[:, h * 128:(h + 1) * 128], AF.Square, accum=s[:, h:h + 1])
                rsq(s[:, :], s[:, :], 1.0 / 128, EPS)
                for h in range(4):
                    stt(a[:, h * 128:(h + 1) * 128], a[:, h * 128:(h + 1) * 128], s[:, h:h + 1],
                        gro[:, h * 128:(h + 1) * 128], ALU.mult, ALU.mult)
                act(z[:, :], z[:, :], AF.Silu)
                tt(a[:, :], a[:, :], z[:, :], ALU.mult)
                y = yd.next()
                ps = psr.next()
                for h in range(4):
                    tr(V(ps.t[:, h * 128:(h + 1) * 128], ps.name), a[:, h * 128:(h + 1) * 128])
                cp(y[:, :, :], V(ps.t[:, :].rearrange("p (a b) -> p a b", a=4), ps.name), "dve")
                st(YS[:, 12:16, b0:b0 + 128], y[:, :, :])

        checkpoint("dn3_%d" % l, [("YS", YS[:, :, :])])
        moe = (l % 2 == 1)
        with Phase() as ph:
            xt = ph.sb("xt", [128, KC, TT], F32, 1)
            hT = ph.sb("hT", [128, KC, TT], BF16, 1)
            ymg = ph.sb("ymg", [128, 2 * KC, TT], BF16, 1)

            class _Sub:
                def __init__(self, off):
                    self.off = off

                def __getitem__(self, idx):
                    return ymg[(idx[0], idx[1] + self.off) + tuple(idx[2:])]
            yT, mg = _Sub(0), _Sub(KC)
            wbig = Rot([ph.sb("wbig%d" % i, [128, KC, 512], BF16) for i in range(3)])
            wsml = Rot([ph.sb("wsml%d" % i, [128, 2, D], BF16) for i in range(3)])
            sgm = Rot([ph.sb("sgm%d" % i, [128, TT]) for i in range(3)])
            macc = ph.sb("macc", [128, TT])
            sq = Rot([ph.sb("sq%d" % i, [128, TT]) for i in range(2)])
            tmp = Rot([ph.sb("tmp%d" % i, [128, TT]) for i in range(2)])
            rstd = ph.sb("rstd", [128, TT])
            acb = Rot([ph.sb("acb%d" % i, [128, TT], BF16) for i in range(4)])
            if moe:
                hfv = ymg.t[:, :, :].rearrange("p (k two) t -> p k (two t)", two=2).bitcast(F32)

                class _HF:
                    def __getitem__(self, idx):
                        kc = idx[1]
                        return V(hfv[(idx[0], kc) + tuple(idx[2:])], [(ymg.name, 2 * kc), (ymg.name, 2 * kc + 1)])
                hf32 = _HF()
                wr = ph.sb("wr", [128, KC, N_EXP])
                ld(wr[:, :, :], moe_wr)
                brr = ph.sb("brr", [128, N_EXP])
                ld(brr[:, :], moe_br.partition_broadcast(128) if False else moe_br[0].partition_broadcast(128))
                lg = ph.sb("lg", [128, 4, N_EXP])
                l2 = ph.sb("l2", [128, 4, N_EXP])
                mk1 = ph.sb("mk1", [128, 4, N_EXP])
                mk2 = ph.sb("mk2", [128, 4, N_EXP])
                m12 = ph.sb("m12", [128, 4, 4])
                gate = ph.sb("gate", [128, 4, N_EXP])
                gbt = Rot([ph.sb("gbt%d" % i, [128, 128]) for i in range(2)])
                GB = ph.sb("GB", [128, N_EXP, TT], F32, 1)
            for t in range(NTILE):
                t0 = t * TT
                grp = 0 if t0 < NSEQ_P * LP else 1
                for kc in range(KC):
                    ld(hT[:, kc, :], HT[:, kc, t0:t0 + TT], "sp")
                    ld(yT[:, kc, :], YS[:, kc, t0:t0 + TT], "act")
                    ld(xt[:, kc, :], XT[:, kc, t0:t0 + TT], "sp")
                for dc in range(KC):
                    gw_ = wbig.next()
                    bw_ = wsml.next()
                    g4 = gw_.t[:, :, :].rearrange("p k (a b) -> p k a b", a=4)
                    b4 = bw_.t[:, 0, :].rearrange("p (k a b) -> p k a b", k=4, a=4)

                    class _G:
                        def __getitem__(self, idx):
                            return V(g4[idx], gw_.name)

                    class _B:
                        def __getitem__(self, idx):
                            return V(b4[idx], bw_.name)
                    g_, b_ = _G(), _B()
                    for k in range(4):
                        c0 = OFF_GATE + k * D + dc * 128
                        ld(g_[:, :, k, :], wslice(w_in[l], c0, c0 + 128), "pool")
                        ld(b_[:, :, k, :], wslice(w_br[l, k], dc * 128, (dc + 1) * 128, 0, 4), "pool")
                    for k in range(4):
                        pg = psr.next()
                        for kc in range(KC):
                            mm(pg[:, :], g_[:, kc, k, :], hT[:, kc, :], start=(kc == 0), stop=(kc == KC - 1))
                        pb = psr.next()
                        for kc in range(4):
                            mm(pb[:, :], b_[:, kc, k, :], yT[:, 4 * k + kc, :], start=(kc == 0), stop=(kc == 3))
                        s = sgm.next()
                        act(s[:, :], pg[:, :], AF.Sigmoid)
                        if k == 0:
                            tt(macc[:, :], s[:, :], pb[:, :], ALU.mult)
                        else:
                            tt(s[:, :], s[:, :], pb[:, :], ALU.mult)
                            if k < 3:
                                tt(macc[:, :], macc[:, :], s[:, :], ALU.add, eng="pool")
                            else:
                                tt(mg[:, dc, :], macc[:, :], s[:, :], ALU.add, eng="pool")
                for g in range(4):
                    w = wbig.next()
                    ld(w[:, :, :], wslice(w_out[l], g * 512, (g + 1) * 512), "pool")
                    for j in range(4):
                        dc = g * 4 + j
                        ps = psr.next()
                        for kc in range(KC):
                            mm(ps[:, :], w[:, kc, j * 128:(j + 1) * 128], mg[:, kc, :], start=(kc == 0),
                               stop=(kc == KC - 1))
                        stt(xt[:, dc, :], ps[:, :], MG1[:, dc, grp:grp + 1], xt[:, dc, :], ALU.mult, ALU.add)
                rms_tile((sq, rstd, tmp), xt, t, MA2, MB2, grp, hT, hf32 if moe else None)
                if moe:
                    pl_ = psr.next()
                    for tb in range(4):
                        for kc in range(KC):
                            mm(V(pl_.t[:, tb * 8:(tb + 1) * 8], pl_.name), hf32[:, kc, tb * 128:(tb + 1) * 128],
                               wr[:, kc, :], start=(kc == 0), stop=(kc == KC - 1))
                    p3 = pl_.t[:, 0:32].rearrange("p (a b) -> p a b", a=4)
                    tt(lg[:, :, :], V(p3, pl_.name),
                       V(brr.t[:, :].unsqueeze(1).broadcast_to([128, 4, N_EXP]), brr.name), ALU.add)
                    P.op("dve", lambda e: e.tensor_reduce(m12.t[:, :, 0:1], lg.t[:, :, :], AX.X, ALU.max),
                         [lg.name], [m12.name])
                    tt(mk1[:, :, :], lg[:, :, :], V(m12.t[:, :, 0:1].broadcast_to([128, 4, N_EXP]), m12.name),
                       ALU.is_equal)
                    stt(l2[:, :, :], mk1[:, :, :], -1e30, lg[:, :, :], ALU.mult, ALU.add)
                    P.op("dve", lambda e: e.tensor_reduce(m12.t[:, :, 1:2], l2.t[:, :, :], AX.X, ALU.max),
                         [l2.name, m12.name], [m12.name])
                    tt(mk2[:, :, :], l2[:, :, :], V(m12.t[:, :, 1:2].broadcast_to([128, 4, N_EXP]), m12.name),
                       ALU.is_equal)
                    tt(m12[:, :, 2:3], m12[:, :, 0:1], m12[:, :, 1:2], ALU.subtract)
                    act(m12[:, :, 2:3], m12[:, :, 2:3], AF.Sigmoid)
                    ts(m12[:, :, 3:4], m12[:, :, 2:3], -1.0, ALU.mult, 1.0, ALU.add)
                    tt(mk1[:, :, :], mk1[:, :, :], V(m12.t[:, :, 2:3].broadcast_to([128, 4, N_EXP]), m12.name),
                       ALU.mult)
                    tt(mk2[:, :, :], mk2[:, :, :], V(m12.t[:, :, 3:4].broadcast_to([128, 4, N_EXP]), m12.name),
                       ALU.mult)
                    tt(gate[:, :, :], mk1[:, :, :], mk2[:, :, :], ALU.add)
                    for e_ in range(N_EXP):
                        for tb in range(4):
                            gb_ = gbt.next()
                            ts(gb_[:, :], ONES, gate[:, tb, e_:e_ + 1], ALU.mult)
                            p = psq()
                            mm(p, gb_[:, :], IDENT)
                            act(GB[:, e_, tb * 128:(tb + 1) * 128], p, AF.Identity)
                nexp = N_EXP if moe else 1
                dff = D_FFE if moe else D_FF
                for e_ in range(nexp):
                    W1 = moe_w1[0, e_] if moe else ffn_w1[0]
                    W3 = moe_w3[0, e_] if moe else ffn_w3[0]
                    W2 = moe_w2[0, e_] if moe else ffn_w2[0]
                    for g in range(dff // 256):
                        a13, a2 = wbig.next(), wsml.next()

                        class _A:
                            def __init__(self, off):
                                self.off = off

                            def __getitem__(self, idx):
                                sl = idx[2]
                                if sl == slice(None):
                                    sl = slice(0, 256)
                                return V(a13.t[idx[0], idx[1], sl.start + self.off:sl.stop + self.off], a13.name)
                        a1, a3 = _A(0), _A(256)
                        ld(a1[:, :, :], wslice(W1, g * 256, (g + 1) * 256), "pool")
                        ld(a3[:, :, :], wslice(W3, g * 256, (g + 1) * 256), "pool")
                        ld(a2[:, :, :], W2[g * 256:(g + 1) * 256, :].rearrange("(k p) n -> p k n", p=128), "pool")
                        acs = []
                        for j in range(2):
                            pa = psr.next()
                            for kc in range(KC):
                                mm(pa[:, :], a1[:, kc, j * 128:(j + 1) * 128], hT[:, kc, :], start=(kc == 0),
                                   stop=(kc == KC - 1))
                            pb = psr.next()
                            for kc in range(KC):
                                mm(pb[:, :], a3[:, kc, j * 128:(j + 1) * 128], hT[:, kc, :], start=(kc == 0),
                                   stop=(kc == KC - 1))
                            s = sgm.next()
                            act(s[:, :], pa[:, :], AF.Silu)
                            ab = acb.next()
                            if moe:
                                tt(s[:, :], s[:, :], pb[:, :], ALU.mult)
                                tt(ab[:, :], s[:, :], GB[:, e_, :], ALU.mult, eng="pool")
                            else:
                                tt(ab[:, :], s[:, :], pb[:, :], ALU.mult)
                            acs.append(ab)
                        for dc in range(KC):
                            ps = psr.next()
                            for j in range(2):
                                mm(ps[:, :], a2[:, j, dc * 128:(dc + 1) * 128], acs[j][:, :], start=(j == 0),
                                   stop=(j == 1))
                            stt(xt[:, dc, :], ps[:, :], MG2[:, dc, grp:grp + 1], xt[:, dc, :], ALU.mult, ALU.add)
                for kc in range(KC):
                    st(XT[:, kc, t0:t0 + TT], xt[:, kc, :], "sp" if kc % 2 == 0 else "act")

        checkpoint("ph3_%d" % l, [("XT", XT[:, :, :])])

    with Phase() as ph:
        xt = ph.sb("xt", [128, KC, TT], F32, 1)
        yn = ph.sb("yn", [128, KC, TT], F32, 1)
        sq = Rot([ph.sb("sq%d" % i, [128, TT]) for i in range(2)])
        rstd = ph.sb("rstd", [128, TT])
        yo = Rot([ph.sb("yo%d" % i, [128, D]) for i in range(2)])
        for t in range(NTILE):
            t0 = t * TT
            for kc in range(KC):
                ld(xt[:, kc, :], XT[:, kc, t0:t0 + TT], "sp" if kc % 2 == 0 else "act")
            ps = psr.next()
            for kc in range(KC):
                s = sq.next()
                act(s[:, :], xt[:, kc, :], AF.Square)
                mm(ps[:, :], ONES, s[:, :], start=(kc == 0), stop=(kc == KC - 1))
            rsq(rstd[:, :], ps[:, :], 1.0 / D, EPS)
            for kc in range(KC):
                stt(yn[:, kc, :], xt[:, kc, :], fing[:, kc:kc + 1], rstd[:, :], ALU.mult, ALU.mult)
            for tb in range(4):
                o = yo.next()
                for g in range(4):
                    ps = psr.next()
                    for j in range(4):
                        kc = 4 * g + j
                        tr(V(ps.t[:, j * 128:(j + 1) * 128], ps.name), yn[:, kc, tb * 128:(tb + 1) * 128])
                    if g % 2 == 0:
                        cp(V(o.t[:, g * 512:(g + 1) * 512], o.name), ps[:, :], "dve")
                    else:
                        act(V(o.t[:, g * 512:(g + 1) * 512], o.name), ps[:, :], AF.Identity)
                st(y_out[t0 + tb * 128:t0 + (tb + 1) * 128, :], o[:, :])


def _fm(v):
    v = np.asarray(v, np.float32)
    n = v.shape[-1] // 128
    v = v.reshape(v.shape[:-1] + (n, 128))
    return np.ascontiguousarray(np.moveaxis(v, -1, 0))


def _consts():
    c = np.zeros((128, 9, 128), np.float32)
    i = np.arange(128)
    c[:, 0, :] = np.eye(128)
    c[:, 1, :] = 1.0
    c[:, 2, :] = np.eye(128)[::-1]
    tri = (i[:, None] <= i[None, :]).astype(np.float32)
    c[:, 3, :] = tri
    c[:, 4, :] = tri.T
    c[:, 5, :] = np.where(i[None, :] < i[:, None], 0.0, NEG)
    c[:, 6, :] = np.where(i[None, :] > i[:, None], 0.0, NEG)
    c[:, 7, :] = np.where(i[None, :] >= i[:, None], 0.0, NEG)
    c[:, 8, :] = np.where(i[None, :] <= i[:, None], 0.0, NEG)
    ic = np.zeros((4, LP + 64), np.float32)
    for j, w in enumerate((2, 4, 8, 16)):
        for (off, n) in ((0, LP), (LP, 64)):
            t = np.arange(n)
            lo = np.maximum(t - w // 2, 0)
            hi = np.minimum(t + (w - 1 - w // 2), n - 1)
            ic[j, off:off + n] = 1.0 / (hi - lo + 1)
    return c.reshape(128, 9 * 128), np.ascontiguousarray(np.broadcast_to(ic, (128, 4, LP + 64)))


_NC = None


def kernel(_upto=None, **inp):
    global _NC
    f = lambda k: np.asarray(inp[k], np.float32)
    if _upto is not None:
        nc = build(_upto)
    else:
        if _NC is None:
            _NC = build()
        nc = _NC
    consts, icnt = _consts()
    shared = {
        "consts": consts, "icnt": icnt,
        "ada_w": f("ada_w"), "adab": _fm(f("ada_b")).transpose(1, 0, 2).copy(),
        "ng": np.stack([_fm(f("norm1_g")), _fm(f("norm2_g"))], 2).transpose(1, 0, 2, 3).copy(),
        "w_in": f("w_in"), "rg_wa": f("rg_wa"), "rg_wx": f("rg_wx"), "pool_w": f("pool_w"),
        "pool_sc": _fm(f("pool_scale")).transpose(1, 0, 2).copy(),
        "sgu_rows": np.stack([f("sgu_ln_g"), f("sgu_ln_b"), f("sgu_bs").reshape(DEPTH, 512)], 1).copy(),
        "sgu_wsT": np.ascontiguousarray(f("sgu_ws").transpose(0, 1, 3, 2)),
        "dn_cw": np.ascontiguousarray(f("dn_conv_w").reshape(DEPTH, 2, 4, 12, 128).transpose(0, 4, 1, 3, 2)),
        "dn_rows": np.stack([f("dn_a_log").reshape(DEPTH, 8), f("dn_dt_bias").reshape(DEPTH, 8),
                             np.zeros((DEPTH, 8), np.float32)], 1).copy(),
        "dn_ng": np.ascontiguousarray(np.tile(f("dn_norm_g"), (1, 4))),
        "w_br": f("w_br"), "w_out": f("w_out"), "ffn_w1": f("ffn_w1"), "ffn_w3": f("ffn_w3"), "ffn_w2": f("ffn_w2"),
        "moe_wr": _fm(f("moe_wr")[0].T).copy(), "moe_br": f("moe_br"),
        "moe_w1": f("moe_w1"), "moe_w3": f("moe_w3"), "moe_w2": f("moe_w2"),
        "fin_g": _fm(f("final_g")),
    }
    shared["moe_wr"] = np.ascontiguousarray(f("moe_wr")[0].reshape(KC, 128, N_EXP).transpose(1, 0, 2))
    xp, xs = f("x_prompt"), f("x_sample")
    c, c_ctx = f("c"), f("c_ctx")
    srg, sdn = f("state_rglru"), f("state_delta")
    in_maps = []
    for i in range(8):
        b = i // 4
        m = dict(shared)
        m["xin"] = np.concatenate([xp[4 * i:4 * i + 4].reshape(NSEQ_P * LP, D), xs[b]], 0)
        m["cT"] = np.ascontiguousarray(np.stack([_fm(c_ctx), _fm(c[b])], -1))
        rv = np.zeros((DEPTH, 128, 2, 4, 9), np.float32)
        cwr = f("rg_conv_w").reshape(DEPTH, 2, 4, 4, 128)
        for k in range(4):
            rv[..., k] = cwr[:, :, k].transpose(0, 3, 1, 2)
        for k, nm in ((4, "rg_conv_b"), (5, "rg_ba"), (6, "rg_bx"), (7, "rg_lam")):
            rv[..., k] = f(nm).reshape(DEPTH, 2, 4, 128).transpose(0, 3, 1, 2)
        rv[..., 8] = srg[b].reshape(DEPTH, 2, 4, 128).transpose(0, 3, 1, 2)
        m["rgv"] = rv
        m["dn_s0"] = np.ascontiguousarray(sdn[b])
        in_maps.append({k: v for k, v in m.items() if k in DECLARED})
    import time as _t
    _t0 = _t.time()
    res = run_bass_kernel_spmd(nc, in_maps, core_ids=list(range(8)))
    if _upto is not None:
        print('spmd call s', _t.time() - _t0)
    r = res.results
    if _upto is not None:
        return r
    y_prompt = np.concatenate([r[i]["y_out"][:NSEQ_P * LP].reshape(NSEQ_P, LP, D) for i in range(8)], 0)
    y_sample = np.stack([r[0]["y_out"][NSEQ_P * LP:], r[4]["y_out"][NSEQ_P * LP:]], 0)
    rg = np.concatenate([r[i]["rg_out"] for i in range(8)], 0)
    dn = np.concatenate([r[i]["dn_out"] for i in range(8)], 0)
    return (y_prompt.astype(np.float32), y_sample.astype(np.float32), rg.astype(np.float32), dn.astype(np.float32))
```

```python
import contextlib
import numpy as np
import concourse.bass as bass
import concourse.mybir as mybir
from concourse.bass_utils import run_bass_kernel_spmd

F32 = mybir.dt.float32
BF16 = mybir.dt.bfloat16
ALU = mybir.AluOpType
AF = mybir.ActivationFunctionType
AX = mybir.AxisListType

D = 2048
KC = 16
DEPTH = 2
NSEQ_P = 4
LP = 256
LS = 4096
NT = NSEQ_P * LP + LS
TT = 512
NTILE = NT // TT
NBLK = NT // 128
N_IN = 12816
OFF_GATE = 4624
D_FF = 5632
N_EXP = 8
D_FFE = 2816
EPS = 1e-6
SEQS = [(i * LP, LP, 0) for i in range(NSEQ_P)] + [(NSEQ_P * LP, LS, 1)]
NEG = -30000.0

ENGS = ("pe", "act", "dve", "pool", "sp")
N_DMA_SEMS = 4


class _Op:
    __slots__ = ("eng", "fn", "waits", "is_dma", "dsem", "dcount", "marked", "count")


class Prog:
    def __init__(self, nc):
        self.nc = nc
        self.ops = {e: [] for e in ENGS}
        self.res = {}
        self.dma_rr = {e: 0 for e in ENGS}
        self.dma_counts = {}

    def _add(self, eng, fn, reads, writes, is_dma=False, extra=()):
        op = _Op()
        op.eng, op.fn, op.is_dma, op.marked, op.count = eng, fn, is_dma, False, 0
        op.dsem, op.dcount = None, 0
        def _isps(k):
            return isinstance(k, str) and k.startswith("ps") and k[2:].isdigit()
        reads = [k[0] if isinstance(k, tuple) and _isps(k[0]) else k for k in reads]
        writes = [k[0] if isinstance(k, tuple) and _isps(k[0]) else k for k in writes]
        writes = writes + [k for k in reads if _isps(k)]
        reads = [k for k in reads if not _isps(k)]
        lst = self.ops[eng]
        idx = len(lst)
        deps = set(extra)
        for r in reads:
            st = self.res.get(r)
            if st is not None and st[0] is not None:
                deps.add(st[0])
        for w in writes:
            st = self.res.get(w)
            if st is not None:
                if st[0] is not None:
                    deps.add(st[0])
                for ei in st[1].values():
                    deps.add(ei)
        waits = []
        for (e, i) in deps:
            p = self.ops[e][i]
            if e == eng and not p.is_dma and not is_dma and eng == "pe":
                continue
            p.marked = True
            waits.append((e, i))
        op.waits = waits
        if is_dma:
            k = self.dma_rr[eng] % N_DMA_SEMS
            self.dma_rr[eng] += 1
            c = self.dma_counts.get((eng, k), 0) + 1
            self.dma_counts[(eng, k)] = c
            op.dsem, op.dcount = (eng, k), c
        lst.append(op)
        me = (eng, idx)
        rkey = (eng, op.dsem) if is_dma else eng
        for r in reads:
            st = self.res.setdefault(r, [None, {}])
            st[1][rkey] = me
        for w in writes:
            self.res[w] = [me, {}]
        return me

    def op(self, eng, fn, reads=(), writes=()):
        return self._add(eng, fn, reads, writes)

    def dma(self, eng, out, in_, reads=(), writes=()):
        return self._add(eng, lambda e: e.dma_start(out=out, in_=in_), reads, writes, is_dma=True)

    def barrier(self):
        last = []
        for e in ENGS:
            lst = self.ops[e]
            j = len(lst) - 1
            while j >= 0 and lst[j].is_dma:
                j -= 1
            if j >= 0 and lst[j].fn is not None:
                last.append((e, j))
            seen = set()
            for j in range(len(lst) - 1, -1, -1):
                if lst[j].is_dma and lst[j].dsem not in seen:
                    seen.add(lst[j].dsem)
                    last.append((e, j))
                if len(seen) == N_DMA_SEMS:
                    break
        for e in ENGS:
            self._add(e, None, (), (), extra=last)
        self.res = {}

    def emit(self):
        nc = self.nc
        with contextlib.ExitStack() as st:
            esem = {e: st.enter_context(nc.semaphore("s_" + e)) for e in ENGS}
            dsem = {}
            for key in sorted(self.dma_counts):
                dsem[key] = st.enter_context(nc.semaphore("d_%s%d" % key))
            for e in ENGS:
                c = 0
                for op in self.ops[e]:
                    if op.is_dma:
                        continue
                    if op.marked:
                        c += 1
                    op.count = c
            block = st.enter_context(nc.Block())

            def run(engname):
                def body(eng):
                    seen = {}
                    for op in self.ops[engname]:
                        for (pe_, pi) in op.waits:
                            p = self.ops[pe_][pi]
                            if p.is_dma:
                                key, val, sem = ("d",) + p.dsem, 16 * p.dcount, dsem[p.dsem]
                            else:
                                key, val, sem = ("e", pe_), p.count, esem[pe_]
                            if seen.get(key, 0) >= val:
                                continue
                            seen[key] = val
                            eng.wait_ge(sem, val)
                        if op.fn is None:
                            continue
                        ins = op.fn(eng)
                        if op.is_dma:
                            ins.then_inc(dsem[op.dsem], 16)
                        elif op.marked:
                            ins.then_inc(esem[engname], 1)
                    for (e, k), c in self.dma_counts.items():
                        if e == engname:
                            eng.wait_ge(dsem[(e, k)], 16 * c)
                return body

            block.tensor(run("pe"))
            block.scalar(run("act"))
            block.vector(run("dve"))
            block.gpsimd(run("pool"))
            block.sync(run("sp"))


class V:
    __slots__ = ("ap", "key")

    def __init__(self, ap, key):
        self.ap, self.key = ap, key


class Buf:
    def __init__(self, t, name, kax=None):
        self.t, self.name, self.kax = t, name, kax

    def __getitem__(self, idx):
        if not isinstance(idx, tuple):
            idx = (idx,)
        key = self.name
        if self.kax is not None and len(idx) > self.kax and isinstance(idx[self.kax], int):
            key = (self.name, idx[self.kax])
        return V(self.t[idx], key)

    def all_keys(self, n):
        return [(self.name, i) for i in range(n)]


class Rot:
    def __init__(self, bufs):
        self.bufs, self.i = bufs, 0

    def next(self):
        b = self.bufs[self.i % len(self.bufs)]
        self.i += 1
        return b


DECLARED = []
DBG_OUT = []


class _Stop(Exception):
    pass


def build(upto=None, dbg=()):
    nc = bass.Bass("TRN2", target_bir_lowering=False)
    P = Prog(nc)
    del DECLARED[:]
    del DBG_OUT[:]
    try:
        _build(nc, P, upto)
    except _Stop:
        pass
    for (name, shape, dt) in dbg:
        pass
    P.emit()
    return nc


def _build(nc, P, upto):
    step = [0]

    def checkpoint(tag, dumps=()):
        if upto is not None and tag == upto:
            for (name, v) in dumps:
                o = nc.dram_tensor("dbg_" + name, list(v.ap.shape), v.ap.dtype, kind="ExternalOutput").ap()
                DBG_OUT.append("dbg_" + name)
                P.dma("sp", o, v.ap, keys(v), [])
            raise _Stop()

    class Lazy:
        def __init__(self, name, shape):
            self.name, self.shape, self._ap = name, list(shape), None

        def ap(self):
            if self._ap is None:
                self._ap = nc.dram_tensor(self.name, self.shape, F32, kind="ExternalInput").ap()
                DECLARED.append(self.name)
            return self._ap

        def __getitem__(self, idx):
            return self.ap()[idx]

        def rearrange(self, *a, **k):
            return self.ap().rearrange(*a, **k)

        def partition_broadcast(self, n):
            return self.ap().partition_broadcast(n)

    def din(name, shape):
        return Lazy(name, shape)

    def dout(name, shape):
        return nc.dram_tensor(name, list(shape), F32, kind="ExternalOutput").ap()

    xin = din("xin", [NT, D])
    cT = din("cT", [128, KC, 2])
    consts = din("consts", [128, 9 * 128])
    icnt = din("icnt", [128, 4, LP + 64])
    ada_w = din("ada_w", [DEPTH, D, 6 * D])
    adab = din("adab", [DEPTH, 128, 96])
    ng = din("ng", [DEPTH, 128, 2, KC])
    w_in = din("w_in", [DEPTH, D, N_IN])
    rgv = din("rgv", [DEPTH, 128, 2, 4, 9])
    rg_wa = din("rg_wa", [DEPTH, 2, 4, 128, 128])
    rg_wx = din("rg_wx", [DEPTH, 2, 4, 128, 128])
    pool_w = din("pool_w", [DEPTH, 4, 128, 128])
    pool_sc = din("pool_sc", [DEPTH, 128, 4])
    sgu_rows = din("sgu_rows", [DEPTH, 3, 512])
    sgu_wsT = din("sgu_wsT", [DEPTH, 4, 128, 128])
    dn_cw = din("dn_cw", [DEPTH, 128, 2, 12, 4])
    dn_rows = din("dn_rows", [DEPTH, 3, 8])
    dn_ng = din("dn_ng", [DEPTH, 512])
    dn_s0 = din("dn_s0", [DEPTH, 2, 4, 128, 128])
    w_br = din("w_br", [DEPTH, 4, 512, D])
    w_out = din("w_out", [DEPTH, D, D])
    ffn_w1 = din("ffn_w1", [1, D, D_FF])
    ffn_w3 = din("ffn_w3", [1, D, D_FF])
    ffn_w2 = din("ffn_w2", [1, D_FF, D])
    moe_wr = din("moe_wr", [128, KC, N_EXP])
    moe_br = din("moe_br", [1, N_EXP])
    moe_w1 = din("moe_w1", [1, N_EXP, D, D_FFE])
    moe_w3 = din("moe_w3", [1, N_EXP, D, D_FFE])
    moe_w2 = din("moe_w2", [1, N_EXP, D_FFE, D])
    fin_g = din("fin_g", [128, KC])
    y_out = dout("y_out", [NT, D])
    rg_out = dout("rg_out", [NSEQ_P, DEPTH, 2, 512])
    dn_out = dout("dn_out", [NSEQ_P, DEPTH, 2, 4, 128, 128])
    def scr(name, shape, dt=F32):
        return Buf(nc.dram_tensor(name, list(shape), dt).ap(), name)

    XT = scr("XT", [128, KC, NT])
    HT = scr("HT", [128, KC, NT], BF16)
    FM = scr("FM", [128, 28, NT])
    TMV = scr("TMV", [NT, 512])
    TMZ = scr("TMZ", [NT, 512])
    TMB = scr("TMB", [NT, 16])
    QKN = [scr("QKN%d" % d, [128, 12, NT]) for d in range(2)]
    OSC = [scr("OSC%d" % d, [NT, 512]) for d in range(2)]
    YS = scr("YS", [128, KC, NT], BF16)

    def sb(name, shape, dt=F32, kax=None):
        return Buf(nc.alloc_sbuf_tensor(name, list(shape), dt), name, kax)

    cst = sb("cst", [128, 9, 128])
    IDENT, ONES, JREV, TRIF, TRIB, MBS_F, MBS_B, MBIT_F, MBIT_B = [cst[:, i, :] for i in range(9)]
    for v in (IDENT, ONES, JREV, TRIF, TRIB, MBS_F, MBS_B, MBIT_F, MBIT_B):
        v.key = "cst"
    csT = sb("csT", [128, KC, 2], BF16)
    modv = sb("modv", [128, 96, 2])
    MA1, MB1, MG1, MB2, MA2, MG2 = [sb("m%d" % i, [128, KC, 2]) for i in range(6)]
    ngs = sb("ngs", [128, 2, KC])
    fing = sb("fing", [128, KC])
    PS = [Buf(nc.alloc_psum_tensor("ps%d" % i, [128, 512], F32), "ps%d" % i) for i in range(8)]
    psr = Rot(PS)

    def psq():
        b = psr.next()
        return V(b.t[:, 0:128], b.name)

    def keys(*vs):
        out = []
        for v in vs:
            if isinstance(v, V):
                if isinstance(v.key, list):
                    out.extend(v.key)
                else:
                    out.append(v.key)
        return out

    def mm(out, lhsT, rhs, start=True, stop=True):
        P.op("pe", lambda e: e.matmul(out.ap, lhsT.ap, rhs.ap, start=start, stop=stop), keys(lhsT, rhs), keys(out))

    def tr(out, in_):
        P.op("pe", lambda e: e.transpose(out.ap, in_.ap, IDENT.ap), keys(in_, IDENT), keys(out))

    def act(out, in_, func, bias=None, scale=1.0, accum=None, eng="act"):
        kw = {}
        if bias is not None:
            kw["bias"] = bias.ap if isinstance(bias, V) else bias
        kw["scale"] = scale.ap if isinstance(scale, V) else scale
        if accum is not None:
            kw["accum_out"] = accum.ap
        P.op("act", lambda e: e.activation(out.ap, in_.ap, func, **kw), keys(in_, bias, scale), keys(out, accum))

    def tt(out, a, b, op, eng="dve"):
        P.op(eng, lambda e: e.tensor_tensor(out.ap, a.ap, b.ap, op), keys(a, b), keys(out))

    def ts(out, a, s1, op0, s2=None, op1=None, eng="dve"):
        s1a = s1.ap if isinstance(s1, V) else s1
        s2a = s2.ap if isinstance(s2, V) else s2
        if op1 is None:
            P.op(eng, lambda e: e.tensor_scalar(out.ap, a.ap, s1a, None, op0), keys(a, s1), keys(out))
        else:
            P.op(eng, lambda e: e.tensor_scalar(out.ap, a.ap, s1a, s2a, op0, op1), keys(a, s1, s2), keys(out))

    def stt(out, a, s, b, op0, op1, eng="dve"):
        sa = s.ap if isinstance(s, V) else s
        P.op(eng, lambda e: e.scalar_tensor_tensor(out.ap, a.ap, sa, b.ap, op0, op1), keys(a, s, b), keys(out))

    def rsq(out, in_, scale, eps):
        act(out, in_, AF.Sqrt, bias=eps, scale=scale)
        P.op("dve", lambda e: e.reciprocal(out.ap, out.ap), keys(out), keys(out))

    def log1p_acc(out, u, z, z2, pp):
        ts(z, u, 2.0, ALU.add)
        P.op("dve", lambda e: e.reciprocal(z.ap, z.ap), keys(z), keys(z))
        tt(z, z, u, ALU.mult)
        tt(z2, z, z, ALU.mult)
        ts(pp, z2, 1.0 / 13, ALU.mult, 1.0 / 11, ALU.add)
        for c_ in (1.0 / 9, 1.0 / 7, 1.0 / 5, 1.0 / 3, 1.0):
            tt(pp, pp, z2, ALU.mult)
            ts(pp, pp, c_, ALU.add)
        stt(out, z, 2.0, pp, ALU.mult, ALU.mult)

    def cp(out, in_, eng="dve"):
        P.op(eng, lambda e: e.tensor_copy(out.ap, in_.ap), keys(in_), keys(out))

    def memset(v, val, eng="pool"):
        P.op(eng, lambda e: e.memset(v.ap, val), (), keys(v))

    def ld(out, in_, eng="sp"):
        if isinstance(in_, Lazy):
            in_ = in_.ap()
        P.dma(eng, out.ap, in_.ap if isinstance(in_, V) else in_, keys(in_), keys(out))

    def st(out, in_, eng="sp"):
        P.dma(eng, out.ap if isinstance(out, V) else out, in_.ap, keys(in_), keys(out))

    class Phase:
        n_ph = [0]

        def __enter__(self):
            self.st = contextlib.ExitStack()
            Phase.n_ph[0] += 1
            self.id = Phase.n_ph[0]
            return self

        def sb(self, name, shape, dt=F32, kax=None):
            name = "%s_p%d" % (name, self.id)
            t = self.st.enter_context(nc.sbuf_tensor(name, list(shape), dt))
            return Buf(t, name, kax)

        def __exit__(self, *a):
            P.barrier()
            self.st.close()
            return False

    ld(cst[:, :, :], consts.rearrange("p (a b) -> p a b", a=9))
    ld(fing[:, :], fin_g)
    with Phase() as ph:
        ctf = ph.sb("ctf", [128, KC, 2])
        ld(ctf[:, :, :], cT)
        act(csT[:, :, :], ctf[:, :, :], AF.Silu)

    with Phase() as ph:
        xa = [ph.sb("xa%d" % i, [128, D]) for i in range(2)]
        xtb = [ph.sb("xtb%d" % i, [128, KC, 128]) for i in range(2)]
        for tb in range(NBLK):
            a, o = xa[tb % 2], xtb[tb % 2]
            ld(a[:, :], xin[tb * 128:(tb + 1) * 128, :], "sp" if tb % 2 == 0 else "act")
            for g in range(4):
                ps = psr.next()
                for j in range(4):
                    tr(V(ps.t[:, j * 128:(j + 1) * 128], ps.name), a[:, (4 * g + j) * 128:(4 * g + j + 1) * 128])
                cp(V(o.t[:, 4 * g:4 * g + 4, :], o.name), V(ps.t[:, :].rearrange("p (a b) -> p a b", a=4), ps.name),
                   "dve")
            st(XT[:, :, tb * 128:(tb + 1) * 128], o[:, :, :])

    checkpoint("p0", [("XT", XT[:, :, :])])

    def wslice(wap, c0, c1, r0=0, nk=KC):
        return wap[r0:r0 + nk * 128, c0:c1].rearrange("(k p) n -> p k n", p=128)

    def rms_tile(ph_bufs, xt, t, MA, MB, grp, hT, hf=None):
        sq, rstd, tmp = ph_bufs
        ps = psr.next()
        for kc in range(KC):
            s = sq.next()
            act(s[:, :], xt[:, kc, :], AF.Square)
            mm(ps[:, :], ONES, s[:, :], start=(kc == 0), stop=(kc == KC - 1))
        rsq(rstd[:, :], ps[:, :], 1.0 / D, EPS)
        for kc in range(KC):
            m = tmp.next()
            tt(m[:, :], xt[:, kc, :], rstd[:, :], ALU.mult)
            if hf is not None:
                act(hf[:, kc, :], m[:, :], AF.Identity, bias=MB[:, kc, grp:grp + 1], scale=MA[:, kc, grp:grp + 1])
                cp(hT[:, kc, :], hf[:, kc, :], "pool")
            else:
                act(hT[:, kc, :], m[:, :], AF.Identity, bias=MB[:, kc, grp:grp + 1], scale=MA[:, kc, grp:grp + 1])

    def gelu_from(ph_b, out, ps, n):
        x, t = ph_b
        act(x[:, 0:n], ps, AF.Identity)
        act(t[:, 0:n], ps, AF.Square)
        ts(t[:, 0:n], t[:, 0:n], 0.044715, ALU.mult, 1.0, ALU.add)
        tt(t[:, 0:n], t[:, 0:n], x[:, 0:n], ALU.mult)
        act(t[:, 0:n], t[:, 0:n], AF.Sigmoid, scale=1.5957691216057308)
        tt(out, t[:, 0:n], x[:, 0:n], ALU.mult)

    for l in range(DEPTH):
        with Phase() as ph:
            was = Rot([ph.sb("wa%d" % i, [128, KC, 512], BF16) for i in range(2)])
            abt = ph.sb("abt", [128, 96])
            ld(abt[:, :], adab[l])
            ld(ngs[:, :, :], ng[l])
            psm = PS[7]
            for g in range(24):
                wa = was.next()
                ld(wa[:, :, :], wslice(ada_w[l], g * 512, (g + 1) * 512), "pool")
                for j in range(4):
                    n = g * 4 + j
                    for kc in range(KC):
                        mm(V(psm.t[:, 2 * n:2 * n + 2], psm.name), wa[:, kc, j * 128:(j + 1) * 128], csT[:, kc, :],
                           start=(kc == 0), stop=(kc == KC - 1))
            ps3 = psm.t[:, 0:192].rearrange("p (a b) -> p a b", b=2)
            for i in range(2):
                tt(modv[:, :, i], V(ps3[:, :, i], psm.name), abt[:, :], ALU.add)
            for i in range(2):
                cp(MB1[:, :, i], modv[:, 0:16, i])
                stt(MA1[:, :, i], modv[:, 16:32, i], 1.0, ngs[:, 0, :], ALU.add, ALU.mult)
                cp(MG1[:, :, i], modv[:, 32:48, i])
                cp(MB2[:, :, i], modv[:, 48:64, i])
                stt(MA2[:, :, i], modv[:, 64:80, i], 1.0, ngs[:, 1, :], ALU.add, ALU.mult)
                cp(MG2[:, :, i], modv[:, 80:96, i])

        checkpoint("mod%d" % l, [("modv", modv[:, :, :]), ("MA1", MA1[:, :, :])])
        with Phase() as ph:
            xt = ph.sb("xt", [128, KC, TT], F32, 1)
            hT = ph.sb("hT", [128, KC, TT], BF16, 1)
            sq = Rot([ph.sb("sq%d" % i, [128, TT]) for i in range(2)])
            tmp = Rot([ph.sb("tmp%d" % i, [128, TT]) for i in range(2)])
            rstd = ph.sb("rstd", [128, TT])
            wts = Rot([ph.sb("w%d" % i, [128, KC, 512], BF16) for i in range(3)])
            stg = Rot([ph.sb("stg%d" % i, [128, TT]) for i in range(3)])
            gx = ph.sb("gx", [128, TT])
            gt = ph.sb("gt", [128, TT])
            for t in range(NTILE):
                t0 = t * TT
                grp = 0 if t0 < NSEQ_P * LP else 1
                for kc in range(KC):
                    ld(xt[:, kc, :], XT[:, kc, t0:t0 + TT], "sp" if kc % 2 == 0 else "act")
                rms_tile((sq, rstd, tmp), xt, t, MA1, MB1, grp, hT)
                for kc in range(KC):
                    st(HT[:, kc, t0:t0 + TT], hT[:, kc, :])
                for (c0, fb, gel) in ((0, 0, 0), (512, 4, 1), (1024, 8, 0), (1536, 12, 1), (2560, 16, 0),
                                      (3072, 20, 0), (3584, 24, 0)):
                    w = wts.next()
                    ld(w[:, :, :], wslice(w_in[l], c0, c0 + 512), "pool")
                    for j in range(4):
                        ps = psr.next()
                        for kc in range(KC):
                            mm(ps[:, :], w[:, kc, j * 128:(j + 1) * 128], hT[:, kc, :], start=(kc == 0),
                               stop=(kc == KC - 1))
                        s = stg.next()
                        if gel:
                            gelu_from((gx, gt), s[:, :], ps[:, :], TT)
                        else:
                            act(s[:, :], ps[:, :], AF.Identity)
                        st(FM[:, fb + j, t0:t0 + TT], s[:, :])
                for (c0, ncol, dst, gel) in ((2048, 512, TMV, 1), (4096, 512, TMZ, 0), (4608, 16, TMB, 0)):
                    w = wts.next()
                    ld(V(w.t[:, :, 0:ncol], w.name), wslice(w_in[l], c0, c0 + ncol), "pool")
                    for tb in range(TT // 128):
                        ps = psr.next()
                        for kc in range(KC):
                            mm(V(ps.t[:, 0:ncol], ps.name), hT[:, kc, tb * 128:(tb + 1) * 128],
                               V(w.t[:, kc, 0:ncol], w.name), start=(kc == 0), stop=(kc == KC - 1))
                        s = stg.next()
                        if gel:
                            gelu_from((gx, gt), V(s.t[:, 0:ncol], s.name), V(ps.t[:, 0:ncol], ps.name), ncol)
                        else:
                            act(V(s.t[:, 0:ncol], s.name), V(ps.t[:, 0:ncol], ps.name), AF.Identity)
                        st(V(dst.t[t0 + tb * 128:t0 + (tb + 1) * 128, :], dst.name), V(s.t[:, 0:ncol], s.name))

        checkpoint("ph1_%d" % l, [("HT", HT[:, :, :]), ("FM", FM[:, :, :]), ("TMV", TMV[:, :]), ("TMZ", TMZ[:, :]), ("TMB", TMB[:, :])])
        with Phase() as ph:
            LM = LS
            xr = ph.sb("xr", [128, LM])
            xv = ph.sb("xv", [128, LM])
            xc = ph.sb("xc", [128, LM])
            rr = ph.sb("rr", [128, LM])
            ii = ph.sb("ii", [128, LM])
            hf = ph.sb("hf", [128, LM])
            hb = ph.sb("hb", [128, LM])
            yb16 = ph.sb("yb16", [128, LM], BF16)
            rt = Rot([ph.sb("rt%d" % i, [128, 128]) for i in range(2)])
            rv = ph.sb("rv", [128, 2, 4, 9])
            wab = ph.sb("wab", [128, 2, 4, 128])
            wxb = ph.sb("wxb", [128, 2, 4, 128])
            c1 = ph.sb("c1", [128, 2, 4])
            zero1 = ph.sb("zero1", [128, 1])
            memset(zero1[:, :], 0.0)
            ld(rv[:, :, :, :], rgv[l])
            ld(wab[:, :, :, :], rg_wa[l].rearrange("d h i j -> i d h j"))
            ld(wxb[:, :, :, :], rg_wx[l].rearrange("d h i j -> i d h j"))
            lz = [ph.sb("lz%d" % i, [128, 2, 4]) for i in range(3)]
            act(c1[:, :, :], rv[:, :, :, 7], AF.Exp, scale=-1.0)
            log1p_acc(c1[:, :, :], c1[:, :, :], lz[0][:, :, :], lz[1][:, :, :], lz[2][:, :, :])
            ts(c1[:, :, :], c1[:, :, :], -8.0, ALU.mult)

            def reverse(dst, src, L):
                nb = L // 128
                for b in range(nb):
                    p1 = psq()
                    tr(p1, V(src.t[:, b * 128:(b + 1) * 128], src.name))
                    r = rt.next()
                    cp(r[:, :], p1, "dve")
                    p2 = psq()
                    mm(p2, r[:, :], JREV)
                    act(V(dst.t[:, (nb - 1 - b) * 128:(nb - b) * 128], dst.name), p2, AF.Identity)

            def rg_dir(src, dst, L, d, h, h0):
                sc = lambda k: rv[:, d, h, k:k + 1]
                X = lambda a, b: V(src.t[:, a:b], src.name)
                C = lambda a, b: V(xc.t[:, a:b], xc.name)
                ts(C(0, L), X(0, L), sc(3), ALU.mult, sc(4), ALU.add)
                for k in (1, 2, 3):
                    stt(C(k, L), X(0, L - k), sc(3 - k), C(k, L), ALU.mult, ALU.add)
                for t0 in range(0, L, TT):
                    n = min(TT, L - t0)
                    ps = psr.next()
                    mm(V(ps.t[:, 0:n], ps.name), wab[:, d, h, :], C(t0, t0 + n))
                    act(V(rr.t[:, t0:t0 + n], rr.name), V(ps.t[:, 0:n], ps.name), AF.Sigmoid, bias=sc(5))
                    ps = psr.next()
                    mm(V(ps.t[:, 0:n], ps.name), wxb[:, d, h, :], C(t0, t0 + n))
                    act(V(ii.t[:, t0:t0 + n], ii.name), V(ps.t[:, 0:n], ps.name), AF.Sigmoid, bias=sc(6))
                R = V(rr.t[:, 0:L], rr.name)
                I = V(ii.t[:, 0:L], ii.name)
                act(R, R, AF.Exp, scale=c1[:, d, h:h + 1])
                tt(I, I, C(0, L), ALU.mult)
                tt(C(0, L), R, R, ALU.mult)
                act(C(0, L), C(0, L), AF.Sqrt, bias=1.0, scale=-1.0)
                tt(I, I, C(0, L), ALU.mult)
                h0a = h0.ap
                P.op("dve", lambda e: e.tensor_tensor_scan(dst.t[:, 0:L], rr.t[:, 0:L], ii.t[:, 0:L], h0a, ALU.mult,
                                                            ALU.add), [rr.name, ii.name, h0.key], [dst.name])

            for si, (s0, L, smp) in enumerate(SEQS):
                for h in range(4):
                    ld(V(xr.t[:, 0:L], xr.name), FM[:, h, s0:s0 + L])
                    h0f = rv[:, 0, h, 8:9] if smp else zero1[:, :]
                    h0b = rv[:, 1, h, 8:9] if smp else zero1[:, :]
                    rg_dir(xr, hf, L, 0, h, h0f)
                    reverse(xv, xr, L)
                    rg_dir(xv, hb, L, 1, h, h0b)
                    if not smp:
                        st(rg_out[si, l, 0, h * 128:(h + 1) * 128].rearrange("(p o) -> p o", o=1),
                           V(hf.t[:, L - 1:L], hf.name))
                        st(rg_out[si, l, 1, h * 128:(h + 1) * 128].rearrange("(p o) -> p o", o=1),
                           V(hb.t[:, L - 1:L], hb.name))
                    reverse(xv, hb, L)
                    tt(V(hf.t[:, 0:L], hf.name), V(hf.t[:, 0:L], hf.name), V(xv.t[:, 0:L], xv.name), ALU.add)
                    ld(V(xr.t[:, 0:L], xr.name), FM[:, 4 + h, s0:s0 + L])
                    tt(V(yb16.t[:, 0:L], yb16.name), V(hf.t[:, 0:L], hf.name), V(xr.t[:, 0:L], xr.name), ALU.mult)
                    st(YS[:, h, s0:s0 + L], V(yb16.t[:, 0:L], yb16.name))

        checkpoint("rg%d" % l, [("YS", YS[:, :, :])])
        with Phase() as ph:
            ic = ph.sb("ic", [128, 4, LP + 64])
            ld(ic[:, :, :], icnt)
            pw = ph.sb("pw", [128, 4, 128])
            ld(pw[:, :, :], pool_w[l].rearrange("g c d -> c g d"))
            psc = ph.sb("psc", [128, 4])
            ld(psc[:, :], pool_sc[l])
            WP, WS = LP + 16, 64 + 16
            pbuf = [ph.sb("pb%d" % i, [128, max(2 * WP, 8 * WS)]) for i in range(5)]
            pl = Rot([ph.sb("pl%d" % i, [128, TT]) for i in range(2)])
            py = Rot([ph.sb("py%d" % i, [128, TT], BF16) for i in range(2)])
            for b_ in pbuf:
                memset(b_[:, :], 0.0)
            for t in range(NTILE):
                t0 = t * TT
                smp = t0 >= NSEQ_P * LP
                R, W = (64, WS) if smp else (LP, WP)
                nr = TT // R
                if t0 == NSEQ_P * LP:
                    memset(pbuf[0][:, :], 0.0)
                for j in range(4):
                    v3 = lambda k: pbuf[k].t[:, 0:nr * W].rearrange("p (r w) -> p r w", w=W)
                    xin_v = V(v3(0)[:, :, 8:8 + R], pbuf[0].name)
                    ld(xin_v, V(FM.t[:, 8 + j, t0:t0 + TT].rearrange("p (r w) -> p r w", w=R), FM.name))
                    sh = [1, 1, 2, 4]
                    lo = [1, 2, 4, 8]
                    for lev in range(j + 1):
                        src = v3(lev)
                        dstv = v3(lev + 1)
                        a, s = lo[lev], sh[lev]
                        n = W - 2 * a + 1 if lev > 0 else W - 1
                        if lev == 0:
                            tt(V(dstv[:, :, 1:W], pbuf[1].name), V(src[:, :, 0:W - 1], pbuf[0].name),
                               V(src[:, :, 1:W], pbuf[0].name), ALU.add)
                        else:
                            tt(V(dstv[:, :, a:W - a + 1], pbuf[lev + 1].name),
                               V(src[:, :, a - s:W - a + 1 - s], pbuf[lev].name),
                               V(src[:, :, a + s:W - a + 1 + s], pbuf[lev].name), ALU.add)
                    p_ = pl.next()
                    p3 = p_.t[:, :].rearrange("p (r w) -> p r w", w=R)
                    ioff = LP if smp else 0
                    icv = V(ic.t[:, j, ioff:ioff + R].unsqueeze(1).broadcast_to([128, nr, R]), ic.name)
                    tt(V(p3, p_.name), V(v3(j + 1)[:, :, 8:8 + R], pbuf[j + 1].name), icv, ALU.mult)
                    tt(V(p3, p_.name), V(p3, p_.name), xin_v, ALU.subtract)
                    ps = psr.next()
                    mm(ps[:, :], pw[:, j, :], p_[:, :])
                    y = py.next()
                    act(y[:, :], ps[:, :], AF.Identity, scale=psc[:, j:j + 1])
                    st(YS[:, 4 + j, t0:t0 + TT], y[:, :])

        checkpoint("pool%d" % l, [("YS", YS[:, :, :])])
        with Phase() as ph:
            rows = ph.sb("rows", [128, 3, 512])
            ld(rows[:, :, :], sgu_rows[l].partition_broadcast(128))
            wsT = ph.sb("wsT", [128, 4, 128])
            ld(wsT[:, :, :], sgu_wsT[l].rearrange("h p q -> p h q"))
            vt = Rot([ph.sb("vt%d" % i, [128, 512]) for i in range(2)])
            ut = Rot([ph.sb("ut%d" % i, [128, 4, 128]) for i in range(2)])
            cen = ph.sb("cen", [128, 512])
            sqv = ph.sb("sqv", [128, 512])
            st1 = Rot([ph.sb("st1%d" % i, [128, 2]) for i in range(2)])
            sy = Rot([ph.sb("sy%d" % i, [128, 4, 128]) for i in range(2)])
            syb = Rot([ph.sb("syb%d" % i, [128, 4, 128], BF16) for i in range(2)])
            for b in range(NBLK):
                b0 = b * 128
                v = vt.next()
                u = ut.next()
                ld(v[:, :], V(TMV.t[b0:b0 + 128, :], TMV.name))
                ld(u[:, :, :], FM[:, 12:16, b0:b0 + 128], "act")
                s1 = st1.next()
                P.op("dve", lambda e, s1=s1, v=v: e.tensor_reduce(s1.t[:, 0:1], v.t[:, :], AX.X, ALU.add),
                     [v.name], [s1.name])
                ts(s1[:, 0:1], s1[:, 0:1], -1.0 / 512, ALU.mult)
                act(cen[:, :], v[:, :], AF.Identity, bias=s1[:, 0:1])
                act(sqv[:, :], cen[:, :], AF.Square, accum=s1[:, 1:2])
                rsq(s1[:, 1:2], s1[:, 1:2], 1.0 / 512, EPS)
                stt(cen[:, :], cen[:, :], s1[:, 1:2], rows[:, 0, :], ALU.mult, ALU.mult)
                tt(cen[:, :], cen[:, :], rows[:, 1, :], ALU.add)
                y = sy.next()
                yb = syb.next()
                for h in range(4):
                    p = psq()
                    mm(p, cen[:, h * 128:(h + 1) * 128], wsT[:, h, :])
                    tt(y[:, h, :], p, rows[:, 2, h * 128:(h + 1) * 128], ALU.add)
                tt(yb[:, :, :], y[:, :, :], u[:, :, :], ALU.mult)
                st(YS[:, 8:12, b0:b0 + 128], yb[:, :, :])

        checkpoint("sgu%d" % l, [("YS", YS[:, :, :])])
        with Phase() as ph:
            cw = ph.sb("cw", [128, 2, 12, 4])
            ld(cw[:, :, :, :], dn_cw[l])
            xb = Rot([ph.sb("xb%d" % i, [128, TT + 6]) for i in range(2)])
            cv = Rot([ph.sb("cv%d" % i, [128, TT]) for i in range(2)])
            sg = Rot([ph.sb("sg%d" % i, [128, TT]) for i in range(2)])
            sq2 = Rot([ph.sb("sq2%d" % i, [128, TT]) for i in range(2)])
            rs = Rot([ph.sb("rs%d" % i, [128, TT]) for i in range(2)])
            for x_ in xb.bufs:
                memset(x_[:, :], 0.0)
            for (s0, L, smp) in SEQS:
                CT = min(TT, L)
                for t0 in range(s0, s0 + L, CT):
                    for c in range(12):
                        x_ = xb.next()
                        lo_ = max(s0, t0 - 3)
                        hi_ = min(s0 + L, t0 + CT + 3)
                        if lo_ > t0 - 3:
                            memset(V(x_.t[:, 0:3], x_.name), 0.0)
                        if hi_ < t0 + CT + 3:
                            memset(V(x_.t[:, CT + 3:CT + 6], x_.name), 0.0)
                        ld(V(x_.t[:, 3 - (t0 - lo_):3 + (hi_ - t0)], x_.name), FM[:, 16 + c, lo_:hi_],
                           "sp" if c % 2 == 0 else "act")
                        for d in range(2):
                            o = cv.next()
                            O = V(o.t[:, 0:CT], o.name)
                            xs = lambda k: V(x_.t[:, 3 + k:3 + k + CT], x_.name)
                            wj = lambda j: cw[:, d, c, j:j + 1]
                            sgn = -1 if d == 0 else 1
                            ts(O, xs(0), wj(3), ALU.mult)
                            for k in (1, 2, 3):
                                stt(O, xs(sgn * k), wj(3 - k), O, ALU.mult, ALU.add)
                            s_ = sg.next()
                            S_ = V(s_.t[:, 0:CT], s_.name)
                            act(S_, O, AF.Silu)
                            if c < 8:
                                q_ = sq2.next()
                                Q_ = V(q_.t[:, 0:CT], q_.name)
                                act(Q_, S_, AF.Square)
                                ps = psr.next()
                                PSV = V(ps.t[:, 0:CT], ps.name)
                                mm(PSV, ONES, Q_)
                                r_ = rs.next()
                                R_ = V(r_.t[:, 0:CT], r_.name)
                                rsq(R_, PSV, 1.0, EPS)
                                if c < 4:
                                    stt(S_, S_, 128.0 ** -0.5, R_, ALU.mult, ALU.mult)
                                else:
                                    tt(S_, S_, R_, ALU.mult)
                            st(QKN[d][:, c, t0:t0 + CT], S_)

        checkpoint("dn1_%d" % l, [("QKN0", QKN[0][:, :, :]), ("QKN1", QKN[1][:, :, :])])
        with Phase() as ph:
            bat = ph.sb("bat", [128, NBLK, 16])
            ld(bat[:, :, :], V(TMB.t[:, :].rearrange("(b p) c -> p b c", p=128), TMB.name))
            drow = ph.sb("drow", [128, 3, 8])
            ld(drow[:, :, :], dn_rows[l].partition_broadcast(128))
            beta = ph.sb("beta", [128, NBLK, 8])
            gg = ph.sb("gg", [128, NBLK, 8])
            t8 = ph.sb("t8", [128, NBLK, 8])
            ea = ph.sb("ea", [128, 8])
            for d in range(2):
                act(beta[:, :, d * 4:d * 4 + 4], bat[:, :, d * 8:d * 8 + 4], AF.Sigmoid)
                tt(gg[:, :, d * 4:d * 4 + 4], bat[:, :, d * 8 + 4:d * 8 + 8],
                   V(drow.t[:, 1, d * 4:d * 4 + 4].unsqueeze(1).broadcast_to([128, NBLK, 4]), drow.name), ALU.add)
            stt(t8[:, :, :], gg[:, :, :], -1.0, gg[:, :, :], ALU.mult, ALU.max)
            act(t8[:, :, :], t8[:, :, :], AF.Exp, scale=-1.0)
            lz = [ph.sb("lzd%d" % i, [128, NBLK, 8]) for i in range(3)]
            log1p_acc(t8[:, :, :], t8[:, :, :], lz[0][:, :, :], lz[1][:, :, :], lz[2][:, :, :])
            stt(gg[:, :, :], gg[:, :, :], 0.0, t8[:, :, :], ALU.max, ALU.add)
            act(ea[:, :], drow[:, 0, :], AF.Exp)
            ts(ea[:, :], ea[:, :], -1.0, ALU.mult)
            tt(gg[:, :, :], gg[:, :, :], V(ea.t[:, :].unsqueeze(1).broadcast_to([128, NBLK, 8]), ea.name), ALU.mult)

            NI = 8
            S = [ph.sb("S%d" % i, [128, 128]) for i in range(NI)]
            qkv_t = [ph.sb("qkv%d" % i, [128, 3, 128]) for i in range(NI)]
            gcol = [ph.sb("gcol%d" % i, [128, 8]) for i in range(2)]
            sc5 = [ph.sb("sc5%d" % i, [128, 5]) for i in range(NI)]
            gbc = [ph.sb("gbc%d" % i, [128, 128]) for i in range(NI)]
            argn = [ph.sb("argn%d" % i, [128, 128]) for i in range(NI)]
            argq = [ph.sb("argq%d" % i, [128, 128]) for i in range(NI)]
            egr = [ph.sb("egr%d" % i, [128, 128]) for i in range(NI)]
            Nm = [[ph.sb("N%d_%d" % (i, k), [128, 128]) for k in range(2)] for i in range(NI)]
            Xm = [[ph.sb("X%d_%d" % (i, k), [128, 128]) for k in range(2)] for i in range(NI)]
            Tm = [[ph.sb("T%d_%d" % (i, k), [128, 128]) for k in range(2)] for i in range(NI)]
            Rm = [ph.sb("R%d" % i, [128, 256]) for i in range(NI)]
            kend = [ph.sb("kend%d" % i, [128, 128]) for i in range(NI)]
            val = [ph.sb("val%d" % i, [128, 128]) for i in range(NI)]
            kcT = [ph.sb("kcT%d" % i, [128, 128]) for i in range(NI)]
            qgT = [ph.sb("qgT%d" % i, [128, 128]) for i in range(NI)]
            qkm = [ph.sb("qkm%d" % i, [128, 128]) for i in range(NI)]
            uu = [ph.sb("uu%d" % i, [128, 128]) for i in range(NI)]
            oo = [ph.sb("oo%d" % i, [128, 128]) for i in range(NI)]

            for si, (s0, L, smp) in enumerate(SEQS):
                nch = L // 128
                for i in range(NI):
                    d, h = i // 4, i % 4
                    if smp:
                        ld(S[i][:, :], dn_s0[l, d, h], "sp" if i % 2 == 0 else "act")
                    else:
                        memset(S[i][:, :], 0.0)
                for step in range(nch):
                    for d in range(2):
                        ch = step if d == 0 else nch - 1 - step
                        blk = (s0 + ch * 128) // 128
                        TRI = TRIF if d == 0 else TRIB
                        p = psq()
                        mm(V(p.ap[:, 0:4], p.key), TRI, gg[:, blk, d * 4:d * 4 + 4])
                        mm(V(p.ap[:, 4:8], p.key), ONES, gg[:, blk, d * 4:d * 4 + 4])
                        cp(gcol[d][:, :], V(p.ap[:, 0:8], p.key))
                    for i in range(NI):
                        d, h = i // 4, i % 4
                        ch = step if d == 0 else nch - 1 - step
                        tk0 = s0 + ch * 128
                        blk = tk0 // 128
                        TRI = TRIF if d == 0 else TRIB
                        MBS = MBS_F if d == 0 else MBS_B
                        MBIT = MBIT_F if d == 0 else MBIT_B
                        hcol = d * 4 + h
                        bcol = beta[:, blk, hcol:hcol + 1]
                        gc = gcol[d][:, h:h + 1]
                        gtot = gcol[d][:, 4 + h:5 + h]
                        q3 = qkv_t[i]
                        for k in range(3):
                            ld(q3[:, k, :], QKN[d][:, 4 * k + h, tk0:tk0 + 128], "sp" if (i + k) % 2 == 0 else "act")
                        qT, kT, vT = q3[:, 0, :], q3[:, 1, :], q3[:, 2, :]
                        s5 = sc5[i]
                        ts(s5[:, 0:1], gc, -1.0, ALU.mult)
                        act(s5[:, 4:5], gc, AF.Exp)
                        tt(s5[:, 1:2], s5[:, 4:5], bcol, ALU.mult)
                        act(s5[:, 2:3], gc, AF.Exp, bias=gtot, scale=-1.0)
                        act(s5[:, 3:4], gtot, AF.Exp)
                        ts(gbc[i][:, :], ONES, gg[:, blk, hcol:hcol + 1], ALU.mult)
                        pg = psq()
                        mm(pg, gbc[i][:, :], TRI)
                        stt(argn[i][:, :], pg, -1.0, MBS, ALU.mult, ALU.add)
                        tt(argq[i][:, :], pg, MBIT, ALU.add)
                        act(egr[i][:, :], pg, AF.Exp)
                        act(argn[i][:, :], argn[i][:, :], AF.Exp, bias=gc)
                        act(argq[i][:, :], argq[i][:, :], AF.Exp, bias=s5[:, 0:1])
                        pk = psq()
                        mm(pk, kT, kT)
                        N0, X0, T0 = Nm[i][0], Xm[i][0], Tm[i][0]
                        stt(N0[:, :], pk, bcol, argn[i][:, :], ALU.mult, ALU.mult)
                        px = psq()
                        tr(px, N0[:, :])
                        cp(X0[:, :], px, "dve")
                        tt(T0[:, :], IDENT, X0[:, :], ALU.subtract)
                        pq = psq()
                        mm(pq, kT, qT)
                        tt(qkm[i][:, :], pq, argq[i][:, :], ALU.mult)
                        tt(qgT[i][:, :], qT, egr[i][:, :], ALU.mult, eng="pool")
                        pkt = psq()
                        tr(pkt, kT)
                        ts(V(Rm[i].t[:, 128:256], Rm[i].name), pkt, s5[:, 1:2], ALU.mult)
                        act(kend[i][:, :], pkt, AF.Identity, scale=s5[:, 2:3])
                        pvt = psq()
                        tr(pvt, vT)
                        ts(V(Rm[i].t[:, 0:128], Rm[i].name), pvt, bcol, ALU.mult)
                    cur = 0
                    for lev in range(1, 7):
                        nxt = 1 - cur
                        for i in range(NI):
                            Nc, Xc, Tc = Nm[i][cur], Xm[i][cur], Tm[i][cur]
                            Nn, Xn, Tn = Nm[i][nxt], Xm[i][nxt], Tm[i][nxt]
                            pn = psq()
                            mm(pn, Xc[:, :], Nc[:, :])
                            act(Nn[:, :], pn, AF.Identity)
                            if lev < 6:
                                pxx = psq()
                                mm(pxx, Nc[:, :], Xc[:, :])
                                cp(Xn[:, :], pxx, "dve")
                            pt = psq()
                            mm(pt, Nn[:, :], Tc[:, :])
                            tt(Tn[:, :], pt, Tc[:, :], ALU.add)
                        cur = nxt
                    for i in range(NI):
                        d, h = i // 4, i % 4
                        ch = step if d == 0 else nch - 1 - step
                        tk0 = s0 + ch * 128
                        Tf = Tm[i][cur]
                        s5 = sc5[i]
                        pw_ = psr.next()
                        mm(V(pw_.t[:, 0:128], pw_.name), Tf[:, :], V(Rm[i].t[:, 0:128], Rm[i].name))
                        cp(val[i][:, :], V(pw_.t[:, 0:128], pw_.name), "dve")
                        pkc = psq()
                        mm(pkc, V(Rm[i].t[:, 128:256], Rm[i].name), Tf[:, :])
                        act(kcT[i][:, :], pkc, AF.Identity)
                        pu = psq()
                        mm(pu, kcT[i][:, :], S[i][:, :])
                        tt(uu[i][:, :], val[i][:, :], pu, ALU.subtract)
                        po = psq()
                        mm(po, qgT[i][:, :], S[i][:, :], start=True, stop=False)
                        mm(po, qkm[i][:, :], uu[i][:, :], start=False, stop=True)
                        act(oo[i][:, :], po, AF.Identity)
                        st(V(OSC[d].t[tk0:tk0 + 128, h * 128:(h + 1) * 128], OSC[d].name), oo[i][:, :],
                           "sp" if i % 2 == 0 else "act")
                        pS = psq()
                        mm(pS, kend[i][:, :], uu[i][:, :])
                        stt(S[i][:, :], S[i][:, :], s5[:, 3:4], pS, ALU.mult, ALU.add)
                if not smp:
                    for i in range(NI):
                        d, h = i // 4, i % 4
                        st(dn_out[si, l, d, h], S[i][:, :])

        checkpoint("dn2_%d" % l, [("OSC0", OSC[0][:, :]), ("OSC1", OSC[1][:, :])])
        with Phase() as ph:
            gro = ph.sb("gro", [128, 512])
            ld(gro[:, :], dn_ng[l].partition_broadcast(128))
            o0 = Rot([ph.sb("o0%d" % i, [128, 512]) for i in range(2)])
            o1 = Rot([ph.sb("o1%d" % i, [128, 512]) for i in range(2)])
            zz = Rot([ph.sb("zz%d" % i, [128, 512]) for i in range(2)])
            sqd = ph.sb("sqd", [128, 512])
            s4 = Rot([ph.sb("s4%d" % i, [128, 4]) for i in range(2)])
            yd = Rot([ph.sb("yd%d" % i, [128, 4, 128], BF16) for i in range(2)])
            for b in range(NBLK):
                b0 = b * 128
                a, b1, z = o0.next(), o1.next(), zz.next()
                ld(a[:, :], V(OSC[0].t[b0:b0 + 128, :], OSC[0].name))
                ld(b1[:, :], V(OSC[1].t[b0:b0 + 128, :], OSC[1].name), "act")
                ld(z[:, :], V(TMZ.t[b0:b0 + 128, :], TMZ.name))
                tt(a[:, :], a[:, :], b1[:, :], ALU.add)
                s = s4.next()
                for h in range(4):
                    act(sqd[:, h * 128:(h + 1) * 128], a[:, h * 128:(h + 1) * 128], AF.Square, accum=s[:, h:h + 1])
                rsq(s[:, :], s[:, :], 1.0 / 128, EPS)
                for h in range(4):
                    stt(a[:, h * 128:(h + 1) * 128], a[:, h * 128:(h + 1) * 128], s[:, h:h + 1],
                        gro[:, h * 128:(h + 1) * 128], ALU.mult, ALU.mult)
                act(z[:, :], z[:, :], AF.Silu)
                tt(a[:, :], a[:, :], z[:, :], ALU.mult)
                y = yd.next()
                ps = psr.next()
                for h in range(4):
                    tr(V(ps.t[:, h * 128:(h + 1) * 128], ps.name), a[:, h * 128:(h + 1) * 128])
                cp(y[:, :, :], V(ps.t[:, :].rearrange("p (a b) -> p a b", a=4), ps.name), "dve")
                st(YS[:, 12:16, b0:b0 + 128], y[:, :, :])

        checkpoint("dn3_%d" % l, [("YS", YS[:, :, :])])
        moe = (l % 2 == 1)
        with Phase() as ph:
            xt = ph.sb("xt", [128, KC, TT], F32, 1)
            hT = ph.sb("hT", [128, KC, TT], BF16, 1)
            ymg = ph.sb("ymg", [128, 2 * KC, TT], BF16, 1)

            class _Sub:
                def __init__(self, off):
                    self.off = off

                def __getitem__(self, idx):
                    return ymg[(idx[0], idx[1] + self.off) + tuple(idx[2:])]
            yT, mg = _Sub(0), _Sub(KC)
            wbig = Rot([ph.sb("wbig%d" % i, [128, KC, 512], BF16) for i in range(3)])
            wsml = Rot([ph.sb("wsml%d" % i, [128, 2, D], BF16) for i in range(3)])
            sgm = Rot([ph.sb("sgm%d" % i, [128, TT]) for i in range(3)])
            macc = ph.sb("macc", [128, TT])
            sq = Rot([ph.sb("sq%d" % i, [128, TT]) for i in range(2)])
            tmp = Rot([ph.sb("tmp%d" % i, [128, TT]) for i in range(2)])
            rstd = ph.sb("rstd", [128, TT])
            acb = Rot([ph.sb("acb%d" % i, [128, TT], BF16) for i in range(4)])
            if moe:
                hfv = ymg.t[:, :, :].rearrange("p (k two) t -> p k (two t)", two=2).bitcast(F32)

                class _HF:
                    def __getitem__(self, idx):
                        kc = idx[1]
                        return V(hfv[(idx[0], kc) + tuple(idx[2:])], [(ymg.name, 2 * kc), (ymg.name, 2 * kc + 1)])
                hf32 = _HF()
                wr = ph.sb("wr", [128, KC, N_EXP])
                ld(wr[:, :, :], moe_wr)
                brr = ph.sb("brr", [128, N_EXP])
                ld(brr[:, :], moe_br.partition_broadcast(128) if False else moe_br[0].partition_broadcast(128))
                lg = ph.sb("lg", [128, 4, N_EXP])
                l2 = ph.sb("l2", [128, 4, N_EXP])
                mk1 = ph.sb("mk1", [128, 4, N_EXP])
                mk2 = ph.sb("mk2", [128, 4, N_EXP])
                m12 = ph.sb("m12", [128, 4, 4])
                gate = ph.sb("gate", [128, 4, N_EXP])
                gbt = Rot([ph.sb("gbt%d" % i, [128, 128]) for i in range(2)])
                GB = ph.sb("GB", [128, N_EXP, TT], F32, 1)
            for t in range(NTILE):
                t0 = t * TT
                grp = 0 if t0 < NSEQ_P * LP else 1
                for kc in range(KC):
                    ld(hT[:, kc, :], HT[:, kc, t0:t0 + TT], "sp")
                    ld(yT[:, kc, :], YS[:, kc, t0:t0 + TT], "act")
                    ld(xt[:, kc, :], XT[:, kc, t0:t0 + TT], "sp")
                for dc in range(KC):
                    gw_ = wbig.next()
                    bw_ = wsml.next()
                    g4 = gw_.t[:, :, :].rearrange("p k (a b) -> p k a b", a=4)
                    b4 = bw_.t[:, 0, :].rearrange("p (k a b) -> p k a b", k=4, a=4)

                    class _G:
                        def __getitem__(self, idx):
                            return V(g4[idx], gw_.name)

                    class _B:
                        def __getitem__(self, idx):
                            return V(b4[idx], bw_.name)
                    g_, b_ = _G(), _B()
                    for k in range(4):
                        c0 = OFF_GATE + k * D + dc * 128
                        ld(g_[:, :, k, :], wslice(w_in[l], c0, c0 + 128), "pool")
                        ld(b_[:, :, k, :], wslice(w_br[l, k], dc * 128, (dc + 1) * 128, 0, 4), "pool")
                    for k in range(4):
                        pg = psr.next()
                        for kc in range(KC):
                            mm(pg[:, :], g_[:, kc, k, :], hT[:, kc, :], start=(kc == 0), stop=(kc == KC - 1))
                        pb = psr.next()
                        for kc in range(4):
                            mm(pb[:, :], b_[:, kc, k, :], yT[:, 4 * k + kc, :], start=(kc == 0), stop=(kc == 3))
                        s = sgm.next()
                        act(s[:, :], pg[:, :], AF.Sigmoid)
                        if k == 0:
                            tt(macc[:, :], s[:, :], pb[:, :], ALU.mult)
                        else:
                            tt(s[:, :], s[:, :], pb[:, :], ALU.mult)
                            if k < 3:
                                tt(macc[:, :], macc[:, :], s[:, :], ALU.add, eng="pool")
                            else:
                                tt(mg[:, dc, :], macc[:, :], s[:, :], ALU.add, eng="pool")
                for g in range(4):
                    w = wbig.next()
                    ld(w[:, :, :], wslice(w_out[l], g * 512, (g + 1) * 512), "pool")
                    for j in range(4):
                        dc = g * 4 + j
                        ps = psr.next()
                        for kc in range(KC):
                            mm(ps[:, :], w[:, kc, j * 128:(j + 1) * 128], mg[:, kc, :], start=(kc == 0),
                               stop=(kc == KC - 1))
                        stt(xt[:, dc, :], ps[:, :], MG1[:, dc, grp:grp + 1], xt[:, dc, :], ALU.mult, ALU.add)
                rms_tile((sq, rstd, tmp), xt, t, MA2, MB2, grp, hT, hf32 if moe else None)
                if moe:
                    pl_ = psr.next()
                    for tb in range(4):
                        for kc in range(KC):
                            mm(V(pl_.t[:, tb * 8:(tb + 1) * 8], pl_.name), hf32[:, kc, tb * 128:(tb + 1) * 128],
                               wr[:, kc, :], start=(kc == 0), stop=(kc == KC - 1))
                    p3 = pl_.t[:, 0:32].rearrange("p (a b) -> p a b", a=4)
                    tt(lg[:, :, :], V(p3, pl_.name),
                       V(brr.t[:, :].unsqueeze(1).broadcast_to([128, 4, N_EXP]), brr.name), ALU.add)
                    P.op("dve", lambda e: e.tensor_reduce(m12.t[:, :, 0:1], lg.t[:, :, :], AX.X, ALU.max),
                         [lg.name], [m12.name])
                    tt(mk1[:, :, :], lg[:, :, :], V(m12.t[:, :, 0:1].broadcast_to([128, 4, N_EXP]), m12.name),
                       ALU.is_equal)
                    stt(l2[:, :, :], mk1[:, :, :], -1e30, lg[:, :, :], ALU.mult, ALU.add)
                    P.op("dve", lambda e: e.tensor_reduce(m12.t[:, :, 1:2], l2.t[:, :, :], AX.X, ALU.max),
                         [l2.name, m12.name], [m12.name])
                    tt(mk2[:, :, :], l2[:, :, :], V(m12.t[:, :, 1:2].broadcast_to([128, 4, N_EXP]), m12.name),
                       ALU.is_equal)
                    tt(m12[:, :, 2:3], m12[:, :, 0:1], m12[:, :, 1:2], ALU.subtract)
                    act(m12[:, :, 2:3], m12[:, :, 2:3], AF.Sigmoid)
                    ts(m12[:, :, 3:4], m12[:, :, 2:3], -1.0, ALU.mult, 1.0, ALU.add)
                    tt(mk1[:, :, :], mk1[:, :, :], V(m12.t[:, :, 2:3].broadcast_to([128, 4, N_EXP]), m12.name),
                       ALU.mult)
                    tt(mk2[:, :, :], mk2[:, :, :], V(m12.t[:, :, 3:4].broadcast_to([128, 4, N_EXP]), m12.name),
                       ALU.mult)
                    tt(gate[:, :, :], mk1[:, :, :], mk2[:, :, :], ALU.add)
                    for e_ in range(N_EXP):
                        for tb in range(4):
                            gb_ = gbt.next()
                            ts(gb_[:, :], ONES, gate[:, tb, e_:e_ + 1], ALU.mult)
                            p = psq()
                            mm(p, gb_[:, :], IDENT)
                            act(GB[:, e_, tb * 128:(tb + 1) * 128], p, AF.Identity)
                nexp = N_EXP if moe else 1
                dff = D_FFE if moe else D_FF
                for e_ in range(nexp):
                    W1 = moe_w1[0, e_] if moe else ffn_w1[0]
                    W3 = moe_w3[0, e_] if moe else ffn_w3[0]
                    W2 = moe_w2[0, e_] if moe else ffn_w2[0]
                    for g in range(dff // 256):
                        a13, a2 = wbig.next(), wsml.next()

                        class _A:
                            def __init__(self, off):
                                self.off = off

                            def __getitem__(self, idx):
                                sl = idx[2]
                                if sl == slice(None):
                                    sl = slice(0, 256)
                                return V(a13.t[idx[0], idx[1], sl.start + self.off:sl.stop + self.off], a13.name)
                        a1, a3 = _A(0), _A(256)
                        ld(a1[:, :, :], wslice(W1, g * 256, (g + 1) * 256), "pool")
                        ld(a3[:, :, :], wslice(W3, g * 256, (g + 1) * 256), "pool")
                        ld(a2[:, :, :], W2[g * 256:(g + 1) * 256, :].rearrange("(k p) n -> p k n", p=128), "pool")
                        acs = []
                        for j in range(2):
                            pa = psr.next()
                            for kc in range(KC):
                                mm(pa[:, :], a1[:, kc, j * 128:(j + 1) * 128], hT[:, kc, :], start=(kc == 0),
                                   stop=(kc == KC - 1))
                            pb = psr.next()
                            for kc in range(KC):
                                mm(pb[:, :], a3[:, kc, j * 128:(j + 1) * 128], hT[:, kc, :], start=(kc == 0),
                                   stop=(kc == KC - 1))
                            s = sgm.next()
                            act(s[:, :], pa[:, :], AF.Silu)
                            ab = acb.next()
                            if moe:
                                tt(s[:, :], s[:, :], pb[:, :], ALU.mult)
                                tt(ab[:, :], s[:, :], GB[:, e_, :], ALU.mult, eng="pool")
                            else:
                                tt(ab[:, :], s[:, :], pb[:, :], ALU.mult)
                            acs.append(ab)
                        for dc in range(KC):
                            ps = psr.next()
                            for j in range(2):
                                mm(ps[:, :], a2[:, j, dc * 128:(dc + 1) * 128], acs[j][:, :], start=(j == 0),
                                   stop=(j == 1))
                            stt(xt[:, dc, :], ps[:, :], MG2[:, dc, grp:grp + 1], xt[:, dc, :], ALU.mult, ALU.add)
                for kc in range(KC):
                    st(XT[:, kc, t0:t0 + TT], xt[:, kc, :], "sp" if kc % 2 == 0 else "act")

        checkpoint("ph3_%d" % l, [("XT", XT[:, :, :])])

    with Phase() as ph:
        xt = ph.sb("xt", [128, KC, TT], F32, 1)
        yn = ph.sb("yn", [128, KC, TT], F32, 1)
        sq = Rot([ph.sb("sq%d" % i, [128, TT]) for i in range(2)])
        rstd = ph.sb("rstd", [128, TT])
        yo = Rot([ph.sb("yo%d" % i, [128, D]) for i in range(2)])
        for t in range(NTILE):
            t0 = t * TT
            for kc in range(KC):
                ld(xt[:, kc, :], XT[:, kc, t0:t0 + TT], "sp" if kc % 2 == 0 else "act")
            ps = psr.next()
            for kc in range(KC):
                s = sq.next()
                act(s[:, :], xt[:, kc, :], AF.Square)
                mm(ps[:, :], ONES, s[:, :], start=(kc == 0), stop=(kc == KC - 1))
            rsq(rstd[:, :], ps[:, :], 1.0 / D, EPS)
            for kc in range(KC):
                stt(yn[:, kc, :], xt[:, kc, :], fing[:, kc:kc + 1], rstd[:, :], ALU.mult, ALU.mult)
            for tb in range(4):
                o = yo.next()
                for g in range(4):
                    ps = psr.next()
                    for j in range(4):
                        kc = 4 * g + j
                        tr(V(ps.t[:, j * 128:(j + 1) * 128], ps.name), yn[:, kc, tb * 128:(tb + 1) * 128])
                    if g % 2 == 0:
                        cp(V(o.t[:, g * 512:(g + 1) * 512], o.name), ps[:, :], "dve")
                    else:
                        act(V(o.t[:, g * 512:(g + 1) * 512], o.name), ps[:, :], AF.Identity)
                st(y_out[t0 + tb * 128:t0 + (tb + 1) * 128, :], o[:, :])


def _fm(v):
    v = np.asarray(v, np.float32)
    n = v.shape[-1] // 128
    v = v.reshape(v.shape[:-1] + (n, 128))
    return np.ascontiguousarray(np.moveaxis(v, -1, 0))


def _consts():
    c = np.zeros((128, 9, 128), np.float32)
    i = np.arange(128)
    c[:, 0, :] = np.eye(128)
    c[:, 1, :] = 1.0
    c[:, 2, :] = np.eye(128)[::-1]
    tri = (i[:, None] <= i[None, :]).astype(np.float32)
    c[:, 3, :] = tri
    c[:, 4, :] = tri.T
    c[:, 5, :] = np.where(i[None, :] < i[:, None], 0.0, NEG)
    c[:, 6, :] = np.where(i[None, :] > i[:, None], 0.0, NEG)
    c[:, 7, :] = np.where(i[None, :] >= i[:, None], 0.0, NEG)
    c[:, 8, :] = np.where(i[None, :] <= i[:, None], 0.0, NEG)
    ic = np.zeros((4, LP + 64), np.float32)
    for j, w in enumerate((2, 4, 8, 16)):
        for (off, n) in ((0, LP), (LP, 64)):
            t = np.arange(n)
            lo = np.maximum(t - w // 2, 0)
            hi = np.minimum(t + (w - 1 - w // 2), n - 1)
            ic[j, off:off + n] = 1.0 / (hi - lo + 1)
    return c.reshape(128, 9 * 128), np.ascontiguousarray(np.broadcast_to(ic, (128, 4, LP + 64)))


_NC = None


def kernel(_upto=None, **inp):
    global _NC
    f = lambda k: np.asarray(inp[k], np.float32)
    if _upto is not None:
        nc = build(_upto)
    else:
        if _NC is None:
            _NC = build()
        nc = _NC
    consts, icnt = _consts()
    shared = {
        "consts": consts, "icnt": icnt,
        "ada_w": f("ada_w"), "adab": _fm(f("ada_b")).transpose(1, 0, 2).copy(),
        "ng": np.stack([_fm(f("norm1_g")), _fm(f("norm2_g"))], 2).transpose(1, 0, 2, 3).copy(),
        "w_in": f("w_in"), "rg_wa": f("rg_wa"), "rg_wx": f("rg_wx"), "pool_w": f("pool_w"),
        "pool_sc": _fm(f("pool_scale")).transpose(1, 0, 2).copy(),
        "sgu_rows": np.stack([f("sgu_ln_g"), f("sgu_ln_b"), f("sgu_bs").reshape(DEPTH, 512)], 1).copy(),
        "sgu_wsT": np.ascontiguousarray(f("sgu_ws").transpose(0, 1, 3, 2)),
        "dn_cw": np.ascontiguousarray(f("dn_conv_w").reshape(DEPTH, 2, 4, 12, 128).transpose(0, 4, 1, 3, 2)),
        "dn_rows": np.stack([f("dn_a_log").reshape(DEPTH, 8), f("dn_dt_bias").reshape(DEPTH, 8),
                             np.zeros((DEPTH, 8), np.float32)], 1).copy(),
        "dn_ng": np.ascontiguousarray(np.tile(f("dn_norm_g"), (1, 4))),
        "w_br": f("w_br"), "w_out": f("w_out"), "ffn_w1": f("ffn_w1"), "ffn_w3": f("ffn_w3"), "ffn_w2": f("ffn_w2"),
        "moe_wr": _fm(f("moe_wr")[0].T).copy(), "moe_br": f("moe_br"),
        "moe_w1": f("moe_w1"), "moe_w3": f("moe_w3"), "moe_w2": f("moe_w2"),
        "fin_g": _fm(f("final_g")),
    }
    shared["moe_wr"] = np.ascontiguousarray(f("moe_wr")[0].reshape(KC, 128, N_EXP).transpose(1, 0, 2))
    xp, xs = f("x_prompt"), f("x_sample")
    c, c_ctx = f("c"), f("c_ctx")
    srg, sdn = f("state_rglru"), f("state_delta")
    in_maps = []
    for i in range(8):
        b = i // 4
        m = dict(shared)
        m["xin"] = np.concatenate([xp[4 * i:4 * i + 4].reshape(NSEQ_P * LP, D), xs[b]], 0)
        m["cT"] = np.ascontiguousarray(np.stack([_fm(c_ctx), _fm(c[b])], -1))
        rv = np.zeros((DEPTH, 128, 2, 4, 9), np.float32)
        cwr = f("rg_conv_w").reshape(DEPTH, 2, 4, 4, 128)
        for k in range(4):
            rv[..., k] = cwr[:, :, k].transpose(0, 3, 1, 2)
        for k, nm in ((4, "rg_conv_b"), (5, "rg_ba"), (6, "rg_bx"), (7, "rg_lam")):
            rv[..., k] = f(nm).reshape(DEPTH, 2, 4, 128).transpose(0, 3, 1, 2)
        rv[..., 8] = srg[b].reshape(DEPTH, 2, 4, 128).transpose(0, 3, 1, 2)
        m["rgv"] = rv
        m["dn_s0"] = np.ascontiguousarray(sdn[b])
        in_maps.append({k: v for k, v in m.items() if k in DECLARED})
    import time as _t
    _t0 = _t.time()
    res = run_bass_kernel_spmd(nc, in_maps, core_ids=list(range(8)))
    if _upto is not None:
        print('spmd call s', _t.time() - _t0)
    r = res.results
    if _upto is not None:
        return r
    y_prompt = np.concatenate([r[i]["y_out"][:NSEQ_P * LP].reshape(NSEQ_P, LP, D) for i in range(8)], 0)
    y_sample = np.stack([r[0]["y_out"][NSEQ_P * LP:], r[4]["y_out"][NSEQ_P * LP:]], 0)
    rg = np.concatenate([r[i]["rg_out"] for i in range(8)], 0)
    dn = np.concatenate([r[i]["dn_out"] for i in range(8)], 0)
    return (y_prompt.astype(np.float32), y_sample.astype(np.float32), rg.astype(np.float32), dn.astype(np.float32))
```
